# Optimizing a Trainium2 kernel written in Bass

```python
import jax
import jax.numpy as jnp
from jax import lax
import numpy as np

D_MODEL = 2048
BATCH = 8
SEQ = 2048
DEPTH = 2

GRID_W = 64
CTX_LEN = 256
N_HEADS = 16
N_KV_HEADS = 4
HEAD_DIM = D_MODEL // N_HEADS
KV_GROUP = N_HEADS // N_KV_HEADS
AXIS_ROT_DIM = HEAD_DIM // 2
ROPE_THETA = 10000.0
WINDOW = 128
BLOCK = 128
CONV_WIDTH = 3
N_EXPERTS = 16
EC_CAPACITY_FACTOR = 2
D_EXPERT = D_MODEL // 2
NORM_EPS = 1e-6
MASK_VALUE = -1e30
MIXER_ORDER = ('conv', 'attn')
N_MIXERS = 2
N_CONV_LAYERS = (DEPTH + 1) // 2
N_ATTN_LAYERS = DEPTH // 2

kernel_name = 'hybrid_shortconv_swa_ec_moe_dit'


def rms_norm(x, gain):
    xf = x.astype(jnp.float32)
    xf = xf * lax.rsqrt(jnp.mean(xf * xf, axis=-1, keepdims=True) + NORM_EPS)
    return (xf * gain.astype(jnp.float32)).astype(x.dtype)


def _row(v):
    return v[..., None, :]


def modulate(x, gain, shift, scale):
    return rms_norm(x, gain) * (1 + _row(scale)) + _row(shift)


def adaln(cond, w_ada, b_ada):
    return jnp.split(jax.nn.silu(cond) @ w_ada + b_ada, 6, axis=-1)


def axial_rope_angles(rows):
    row = jnp.repeat(jnp.arange(rows, dtype=jnp.float32), GRID_W)
    col = jnp.tile(jnp.arange(GRID_W, dtype=jnp.float32), rows)
    inv_freq = ROPE_THETA ** (-jnp.arange(AXIS_ROT_DIM // 2, dtype=jnp.float32) * 2.0 / AXIS_ROT_DIM)
    return row[:, None] * inv_freq, col[:, None] * inv_freq


def rope_rotate(x, ang):
    d2 = x.shape[-1] // 2
    cos = jnp.cos(ang)[:, None, :]
    sin = jnp.sin(ang)[:, None, :]
    x1, x2 = x[..., :d2], x[..., d2:]
    return jnp.concatenate([x1 * cos - x2 * sin, x2 * cos + x1 * sin], axis=-1)


def axial_rope(x, ang_row, ang_col):
    xf = x.astype(jnp.float32)
    out = jnp.concatenate([rope_rotate(xf[..., :AXIS_ROT_DIM], ang_row),
                           rope_rotate(xf[..., AXIS_ROT_DIM:], ang_col)], axis=-1)
    return out.astype(x.dtype)


def short_gated_conv(h, w_in, conv_w, w_out):
    s = h.shape[1]
    gate_b, gate_c, xv = jnp.split(h @ w_in, 3, axis=-1)
    u = gate_c * xv
    pad = CONV_WIDTH // 2
    up = jnp.pad(u, ((0, 0), (pad, pad), (0, 0)))
    y = conv_w[0] * up[:, 0:s]
    for j in range(1, CONV_WIDTH):
        y = y + conv_w[j] * up[:, j:j + s]
    return (gate_b * y) @ w_out


def windowed_gqa_sink(a_lat, a_ctx, w_qkv, q_gain, k_gain, sink, w_o, ang_row, ang_col, update_ctx):
    b, s, _ = a_lat.shape
    n_ctx = a_ctx.shape[1]
    n_blk = s // BLOCK
    dq = N_HEADS * HEAD_DIM
    dkv = N_KV_HEADS * HEAD_DIM
    scale = HEAD_DIM ** -0.5

    def heads(t, n_heads, gain):
        t = t.reshape(t.shape[0], t.shape[1], n_heads, HEAD_DIM)
        return t if gain is None else rms_norm(t, gain)

    qkv = a_lat @ w_qkv
    q = axial_rope(heads(qkv[..., :dq], N_HEADS, q_gain), ang_row, ang_col)
    k = axial_rope(heads(qkv[..., dq:dq + dkv], N_KV_HEADS, k_gain), ang_row, ang_col)
    v = heads(qkv[..., dq + dkv:], N_KV_HEADS, None)
    kv_ctx = a_ctx @ w_qkv[:, dq:]
    k_ctx = heads(kv_ctx[..., :dkv], N_KV_HEADS, k_gain)
    v_ctx = heads(kv_ctx[..., dkv:], N_KV_HEADS, None)
    sink_f = sink.astype(jnp.float32).reshape(N_KV_HEADS, KV_GROUP)

    qb = q.reshape(b, n_blk, BLOCK, N_KV_HEADS, KV_GROUP, HEAD_DIM)

    def band(t):
        tp = jnp.pad(t, ((0, 0), (BLOCK, BLOCK), (0, 0), (0, 0)))
        tb = tp.reshape(b, n_blk + 2, BLOCK, N_KV_HEADS, HEAD_DIM)
        return jnp.concatenate([tb[:, :-2], tb[:, 1:-1], tb[:, 2:]], axis=2)

    kb, vb = band(k), band(v)
    q_pos = jnp.arange(s).reshape(n_blk, BLOCK)
    k_pos = (jnp.arange(n_blk)[:, None] - 1) * BLOCK + jnp.arange(3 * BLOCK)[None, :]
    valid = ((k_pos[:, None, :] >= 0) & (k_pos[:, None, :] < s)
             & (jnp.abs(q_pos[:, :, None] - k_pos[:, None, :]) <= WINDOW))

    s_loc = jnp.einsum('bnqhgd,bnkhd->bhgnqk', qb, kb, preferred_element_type=jnp.float32) * scale
    s_loc = jnp.where(valid[None, None, None], s_loc, MASK_VALUE)
    s_ctx = jnp.einsum('bnqhgd,bchd->bhgnqc', qb, k_ctx, preferred_element_type=jnp.float32) * scale
    s_sink = jnp.broadcast_to(sink_f[None, :, :, None, None, None], s_loc.shape[:-1] + (1,))
    p = jax.nn.softmax(jnp.concatenate([s_loc, s_ctx, s_sink], axis=-1), axis=-1)
    p_loc = p[..., :3 * BLOCK].astype(v.dtype)
    p_ctx = p[..., 3 * BLOCK:3 * BLOCK + n_ctx].astype(v.dtype)
    o = (jnp.einsum('bhgnqk,bnkhd->bnqhgd', p_loc, vb)
         + jnp.einsum('bhgnqc,bchd->bnqhgd', p_ctx, v_ctx))
    y_lat = o.reshape(b, s, dq) @ w_o

    if not update_ctx:
        return y_lat, None
    q_ctx = heads(a_ctx @ w_qkv[:, :dq], N_HEADS, q_gain).reshape(b, n_ctx, N_KV_HEADS, KV_GROUP, HEAD_DIM)
    sc = jnp.einsum('bqhgd,bchd->bhgqc', q_ctx, k_ctx, preferred_element_type=jnp.float32) * scale
    sc_sink = jnp.broadcast_to(sink_f[None, :, :, None, None], sc.shape[:-1] + (1,))
    pc = jax.nn.softmax(jnp.concatenate([sc, sc_sink], axis=-1), axis=-1)[..., :n_ctx].astype(v_ctx.dtype)
    oc = jnp.einsum('bhgqc,bchd->bqhgd', pc, v_ctx).reshape(b, n_ctx, dq)
    return y_lat, oc @ w_o


def expert_choice_swiglu(h, w_router, w_gate, w_up, w_down):
    b, n, _ = h.shape
    cap = EC_CAPACITY_FACTOR * n // N_EXPERTS
    logits = jnp.einsum('bnd,de->bne', h, w_router, preferred_element_type=jnp.float32)
    affinity = jax.nn.softmax(logits, axis=-1)
    gates, idx = lax.top_k(jnp.swapaxes(affinity, 1, 2), cap)
    b_idx = jnp.arange(b)[:, None, None]
    xs = h[b_idx, idx]
    a = jnp.einsum('becd,edf->becf', xs, w_gate)
    u = jnp.einsum('becd,edf->becf', xs, w_up)
    y = jnp.einsum('becf,efd->becd', jax.nn.silu(a) * u, w_down)
    y = y * gates[..., None].astype(y.dtype)
    return jnp.zeros_like(h).at[b_idx, idx].add(y)


def setup_inputs(seed: int = 0) -> dict:
    key = jax.random.key(seed)
    ks = jax.random.split(key, 24)
    d, f = D_MODEL, D_EXPERT
    dqkv = (N_HEADS + 2 * N_KV_HEADS) * HEAD_DIM
    nrm = jax.random.normal
    return {
        'x': nrm(ks[0], (BATCH, SEQ, d), jnp.float32),
        'c': nrm(ks[1], (BATCH, d), jnp.float32),
        'ctx': nrm(ks[2], (BATCH, CTX_LEN, d), jnp.float32),
        'c_ctx': nrm(ks[3], (d,), jnp.float32),
        'ada_w': nrm(ks[4], (DEPTH, d, 6 * d), jnp.float32) * (0.5 * d ** -0.5),
        'ada_b': nrm(ks[5], (DEPTH, 6 * d), jnp.float32) * 0.02,
        'norm_mix_g': 1.0 + 0.02 * nrm(ks[6], (DEPTH, d), jnp.float32),
        'norm_ffn_g': 1.0 + 0.02 * nrm(ks[7], (DEPTH, d), jnp.float32),
        'conv_w_in': nrm(ks[8], (N_CONV_LAYERS, d, 3 * d), jnp.float32) * d ** -0.5,
        'conv_w': nrm(ks[9], (N_CONV_LAYERS, CONV_WIDTH, d), jnp.float32) * CONV_WIDTH ** -0.5,
        'conv_w_out': nrm(ks[10], (N_CONV_LAYERS, d, d), jnp.float32) * d ** -0.5,
        'attn_w_qkv': nrm(ks[11], (N_ATTN_LAYERS, d, dqkv), jnp.float32) * d ** -0.5,
        'attn_q_gain': 1.0 + 0.02 * nrm(ks[12], (N_ATTN_LAYERS, HEAD_DIM), jnp.float32),
        'attn_k_gain': 1.0 + 0.02 * nrm(ks[13], (N_ATTN_LAYERS, HEAD_DIM), jnp.float32),
        'attn_sink': nrm(ks[14], (N_ATTN_LAYERS, N_HEADS), jnp.float32),
        'attn_w_o': nrm(ks[15], (N_ATTN_LAYERS, N_HEADS * HEAD_DIM, d), jnp.float32) * d ** -0.5,
        'router_w': nrm(ks[16], (DEPTH, d, N_EXPERTS), jnp.float32) * d ** -0.5,
        'expert_w_gate': nrm(ks[17], (DEPTH, N_EXPERTS, d, f), jnp.float32) * d ** -0.5,
        'expert_w_up': nrm(ks[18], (DEPTH, N_EXPERTS, d, f), jnp.float32) * d ** -0.5,
        'expert_w_down': nrm(ks[19], (DEPTH, N_EXPERTS, f, d), jnp.float32) * f ** -0.5,
    }


def reference(x, c, ctx, c_ctx, ada_w, ada_b, norm_mix_g, norm_ffn_g, conv_w_in, conv_w, conv_w_out,
              attn_w_qkv, attn_q_gain, attn_k_gain, attn_sink, attn_w_o, router_w,
              expert_w_gate, expert_w_up, expert_w_down):
    s = x.shape[1]
    rows = s // GRID_W
    ang_row, ang_col = axial_rope_angles(rows)
    h_lat, h_ctx = x, ctx
    i_conv = 0
    i_attn = 0
    for i in range(DEPTH):
        kind = MIXER_ORDER[i % N_MIXERS]
        ctx_live = any(MIXER_ORDER[j % N_MIXERS] == 'attn' for j in range(i + 1, DEPTH))
        sh1, sc1, g1, sh2, sc2, g2 = adaln(c, ada_w[i], ada_b[i])
        csh1, csc1, cg1, csh2, csc2, cg2 = adaln(c_ctx, ada_w[i], ada_b[i])
        a_lat = modulate(h_lat, norm_mix_g[i], sh1, sc1)
        if kind == 'conv':
            y_lat = short_gated_conv(a_lat, conv_w_in[i_conv], conv_w[i_conv], conv_w_out[i_conv])
            y_ctx = None
            if ctx_live:
                a_ctx = modulate(h_ctx, norm_mix_g[i], csh1, csc1)
                y_ctx = short_gated_conv(a_ctx, conv_w_in[i_conv], conv_w[i_conv], conv_w_out[i_conv])
            i_conv += 1
        else:
            a_ctx = modulate(h_ctx, norm_mix_g[i], csh1, csc1)
            y_lat, y_ctx = windowed_gqa_sink(a_lat, a_ctx, attn_w_qkv[i_attn], attn_q_gain[i_attn],
                                             attn_k_gain[i_attn], attn_sink[i_attn], attn_w_o[i_attn],
                                             ang_row, ang_col, ctx_live)
            i_attn += 1
        h_lat = h_lat + _row(g1) * y_lat
        h_lat = h_lat + _row(g2) * expert_choice_swiglu(modulate(h_lat, norm_ffn_g[i], sh2, sc2), router_w[i],
                                                        expert_w_gate[i], expert_w_up[i], expert_w_down[i])
        if ctx_live:
            h_ctx = h_ctx + _row(cg1) * y_ctx
            h_ctx = h_ctx + _row(cg2) * expert_choice_swiglu(modulate(h_ctx, norm_ffn_g[i], csh2, csc2),
                                                             router_w[i], expert_w_gate[i], expert_w_up[i],
                                                             expert_w_down[i])
    return h_lat
```

```python
import contextlib
import numpy as np
import concourse.bass as bass
import concourse.mybir as mybir
from concourse.bass_utils import run_bass_kernel_spmd

F32 = mybir.dt.float32
BF16 = mybir.dt.bfloat16
I32 = mybir.dt.int32
U32 = mybir.dt.uint32
AF = mybir.ActivationFunctionType
ALU = mybir.AluOpType
AX = mybir.AxisListType

D = 2048
S = 2048
NCTX = 256
NT = S + NCTX
NE = 16
CAP = 256
CAPC = 32
FE = 1024
EPS = 1e-6
NCORES = 8


class Op:
    __slots__ = ("eng", "fn", "deps", "needs_inc", "dma", "sem", "val", "idx")

    def __init__(self, eng, fn, dma):
        self.eng = eng
        self.fn = fn
        self.dma = dma
        self.deps = set()
        self.needs_inc = dma
        self.sem = None
        self.val = 0


class Sched:
    ENGS = ("pe", "act", "dve", "pool", "sp")

    def __init__(self, nc, stack, n_dma_sems=(("sp", 8), ("pool", 12), ("act", 4))):
        self.nc = nc
        self.by_eng = {e: [] for e in self.ENGS}
        self.last_w = {}
        self.readers = {}
        self.prog_sem = {}
        for e in ("pe", "act", "dve", "pool"):
            self.prog_sem[e] = stack.enter_context(nc.semaphore("prog_" + e))
        self.dma_sems = {}
        self.dma_rr = {}
        self.dma_last = {}
        for q, n in n_dma_sems:
            self.dma_sems[q] = [stack.enter_context(nc.semaphore("dq_%s_%d" % (q, i))) for i in range(n)]
            self.dma_rr[q] = 0
        self.barrier_deps = {e: set() for e in self.ENGS}
        self.all_since_barrier = []
        self.nops = 0

    def _add(self, eng, fn, reads, writes, dma):
        o = Op(eng, fn, dma)
        self.nops += 1
        deps = o.deps
        for k in reads:
            w = self.last_w.get(k)
            if w is not None:
                deps.add(w)
        for k in writes:
            w = self.last_w.get(k)
            if w is not None:
                deps.add(w)
            rd = self.readers.get(k)
            if rd:
                for r in rd.values():
                    deps.add(r)
        if not dma:
            rset = None
            for d in list(deps):
                if (not d.dma) and d.eng == eng:
                    if eng == "pe":
                        deps.discard(d)
                    elif eng == "pool":
                        pass
                    else:
                        if rset is None:
                            rset = set()
                            for k in reads:
                                w = self.last_w.get(k)
                                if w is not None:
                                    rset.add(w)
                        if d not in rset:
                            deps.discard(d)
        bd = self.barrier_deps[eng]
        if bd:
            deps |= bd
            self.barrier_deps[eng] = set()
        if dma:
            sems = self.dma_sems[eng]
            i = self.dma_rr[eng]
            self.dma_rr[eng] = (i + 1) % len(sems)
            o.sem = sems[i]
            prev = self.dma_last.get((eng, i))
            if prev is not None:
                deps.add(prev)
                o.val = prev.val + 16
            else:
                o.val = 16
            self.dma_last[(eng, i)] = o
        for d in deps:
            d.needs_inc = True
        for k in writes:
            self.last_w[k] = o
            self.readers[k] = {}
        for k in reads:
            rd = self.readers.setdefault(k, {})
            rd[("dma", id(o)) if dma else eng] = o
        self.by_eng[eng].append(o)
        self.all_since_barrier.append(o)
        return o

    def op(self, eng, fn, reads=(), writes=()):
        return self._add(eng, fn, reads, writes, False)

    def dma(self, queue, fn, reads=(), writes=()):
        return self._add(queue, fn, reads, writes, True)

    def barrier(self):
        last = {}
        pend = set()
        for o in self.all_since_barrier:
            if o.dma:
                pend.add(o)
            else:
                last[o.eng] = o
        pend |= set(last.values())
        for e in self.ENGS:
            self.barrier_deps[e] |= pend
        for o in pend:
            o.needs_inc = True
        self.all_since_barrier = []

    def emit(self, block):
        nc = self.nc
        for e in ("pe", "act", "dve", "pool"):
            cnt = 0
            for o in self.by_eng[e]:
                if o.dma:
                    continue
                if o.needs_inc:
                    cnt += 1
                    o.sem = self.prog_sem[e]
                    o.val = cnt
        engmap = {"pe": "tensor", "act": "scalar", "dve": "vector", "pool": "gpsimd", "sp": "sync"}

        def run(e, eng):
            waited = {}
            for o in self.by_eng[e]:
                need = {}
                for d in o.deps:
                    key = id(d.sem)
                    if waited.get(key, 0) < d.val and need.get(key, (None, 0))[1] < d.val:
                        need[key] = (d.sem, d.val)
                for key, (sem, val) in need.items():
                    eng.wait_ge(sem, val)
                    waited[key] = val
                ins = o.fn(eng)
                if o.dma:
                    ins.then_inc(o.sem, 16)
                elif o.needs_inc:
                    ins.then_inc(o.sem, 1)

        for e in self.ENGS:
            if not self.by_eng[e]:
                continue
            deco = getattr(block, engmap[e])

            def body(eng, e=e):
                run(e, eng)

            deco(body)


def _consts():
    c = {}
    c["ident"] = np.eye(128, dtype=np.float32)
    t = np.arange(S)
    row = (t // 64).astype(np.float32)
    col = (t % 64).astype(np.float32)
    inv = (10000.0 ** (-np.arange(32, dtype=np.float32) * 2.0 / 64.0)).astype(np.float32)
    ang = np.zeros((128, S), np.float32)
    for i in range(128):
        pos = row if i < 64 else col
        ang[i] = pos * inv[i % 32]
    c["cosT"] = np.cos(ang).astype(np.float32)
    c["sinT"] = np.sin(ang).astype(np.float32)
    R = np.zeros((128, 128), np.float32)
    for m in range(128):
        if m % 64 < 32:
            R[m + 32, m] = -1.0
        else:
            R[m - 32, m] = 1.0
    c["rotm"] = R
    kk = np.arange(128)[:, None]
    qq = np.arange(128)[None, :]
    mprev = (kk >= qq).astype(np.float32)
    mnext = (kk <= qq).astype(np.float32)
    c["mprev"] = np.tile(mprev, (1, 4))
    c["mnext"] = np.tile(mnext, (1, 4))
    return c


class B:
    def __init__(self, nc, sch):
        self.nc = nc
        self.s = sch

    def mm(self, out, lhsT, rhs, start, stop, reads, writes):
        return self.s.op("pe", lambda e: e.matmul(out, lhsT, rhs, start=start, stop=stop), reads, writes)

    def tr(self, out, in_, ident, reads, writes):
        return self.s.op("pe", lambda e: e.transpose(out, in_, ident), reads, writes)

    def act(self, out, in_, func, reads, writes, **kw):
        return self.s.op("act", lambda e: e.activation(out, in_, func, **kw), reads, writes)

    def tt(self, eng, out, in0, in1, op, reads, writes):
        return self.s.op(eng, lambda e: e.tensor_tensor(out, in0, in1, op), reads, writes)

    def ts(self, eng, out, in0, s1, s2, op0, op1, reads, writes):
        if op1 is None:
            return self.s.op(eng, lambda e: e.tensor_scalar(out, in0, s1, None, op0), reads, writes)
        return self.s.op(eng, lambda e: e.tensor_scalar(out, in0, s1, s2, op0, op1), reads, writes)

    def stt(self, eng, out, in0, scalar, in1, op0, op1, reads, writes):
        return self.s.op(eng, lambda e: e.scalar_tensor_tensor(out, in0, scalar, in1, op0, op1), reads, writes)

    def cp(self, eng, out, in_, reads, writes):
        return self.s.op(eng, lambda e: e.tensor_copy(out, in_), reads, writes)

    def dma(self, q, out, in_, reads, writes, **kw):
        return self.s.dma(q, lambda e: e.dma_start(out=out, in_=in_, **kw), reads, writes)


def build_program(debug=None):
    nc = bass.Bass("TRN2", target_bir_lowering=False)
    dbg = {}

    tiny = ()
    for n_, v_, _ in (debug or []):
        if n_ == "_tiny":
            tiny = v_ if isinstance(v_, (list, tuple)) else (
                "ada_w", "conv_w_in", "conv_w_out", "attn_w_qkv", "attn_w_o", "expert_w_gate",
                "expert_w_up", "expert_w_down", "x", "ctx")

    def din(name, shape, dt=F32):
        if name in tiny:
            shape = [2] * len(shape)
        return nc.dram_tensor(name, list(shape), dt, kind="ExternalInput").ap()

    x = din("x", [S, D])
    ctx = din("ctx", [NCTX, D])
    cc = din("cc", [2, D])
    ada_w = din("ada_w", [2, D, 6 * D])
    ada_b = din("ada_b", [2, 6 * D])
    norm_mix_g = din("norm_mix_g", [2, D])
    norm_ffn_g = din("norm_ffn_g", [2, D])
    conv_w_in = din("conv_w_in", [D, 3 * D])
    conv_w = din("conv_w", [3, D])
    conv_w_out = din("conv_w_out", [D, D])
    w_qkv = din("attn_w_qkv", [D, 3072])
    q_gain = din("attn_q_gain", [128])
    k_gain = din("attn_k_gain", [128])
    sink = din("attn_sink", [16])
    w_o = din("attn_w_o", [D, D])
    router_w = din("router_w", [2, D, NE])
    w_gate = din("expert_w_gate", [2, NE, D, FE])
    w_up = din("expert_w_up", [2, NE, D, FE])
    w_down = din("expert_w_down", [2, NE, FE, D])
    c_ident = din("c_ident", [128, 128])
    c_cos = din("c_cosT", [128, S])
    c_sin = din("c_sinT", [128, S])
    c_rot = din("c_rotm", [128, 128])
    c_mprev = din("c_mprev", [128, 512])
    c_mnext = din("c_mnext", [128, 512])

    out = nc.dram_tensor("out", [S, D], F32, kind="ExternalOutput").ap()
    modD = nc.dram_tensor("modD", [2, 2, 6 * D], F32, kind="Internal").ap()
    hnA = nc.dram_tensor("hnA", [NT, D], F32, kind="Internal").ap()
    ctxB = nc.dram_tensor("ctxB", [NCTX, D], F32, kind="Internal").ap()
    vD = nc.dram_tensor("vD", [D, NT], BF16, kind="Internal").ap()

    if debug:
        for name, shape, dt in debug:
            if name.startswith("_"):
                dbg[name] = shape
                continue
            dbg[name] = nc.dram_tensor("dbg_" + name, list(shape), dt, kind="ExternalOutput").ap()

    with contextlib.ExitStack() as stack:
        sch = Sched(nc, stack)
        b = B(nc, sch)
        ent = stack.enter_context

        def sb(name, shape, dt):
            return ent(nc.sbuf_tensor(name, list(shape), dt))

        ident = sb("ident", [128, 128], F32)
        identb = sb("identb", [128, 128], BF16)
        b.dma("sp", ident[:], c_ident, [], ["ident"])
        b.cp("dve", identb[:], ident[:], ["ident"], ["identb"])
        vec_names = []
        for l in range(2):
            for r in ("lat", "ctx"):
                for nm in ("G1", "S1", "G2", "S2"):
                    vec_names.append("%s_%s_%d" % (nm, r, l))
        PV = {n: sb("pv_" + n, [128, 16], F32) for n in vec_names}
        psum = [ent(nc.psum_tensor("ps%d" % i, [128, 512], F32)) for i in range(8)]

        skip_pre = dbg.get("_skip_pre", False)
        sT = sb("ad_sT", [128, 16, 2], BF16)
        if not skip_pre:
            phase_adaln(nc, b, sb, stack, psum, cc, ada_w, ada_b, modD, ident, dbg, sT, layers=(0,))
        sch.barrier()
        load_mod_vectors(nc, b, sb, modD, norm_mix_g, norm_ffn_g, PV, layers=(0,) if not skip_pre else (0, 1))

        if not skip_pre:
            phase_conv_a(nc, b, psum, x, ctx, conv_w_in, conv_w, vD, ident, PV, dbg)

        def src0(j):
            return (x[j * 128:(j + 1) * 128, :], "x%d" % j) if j < 16 else (ctx[(j - 16) * 128:(j - 15) * 128, :], "cx%d" % j)

        def dst0(j):
            return (out[j * 128:(j + 1) * 128, :], "out%d" % j) if j < 16 else (ctxB[(j - 16) * 128:(j - 15) * 128, :], "ctxB%d" % j)

        if not skip_pre:
            phase_proj_out(nc, b, psum, vD, NT, conv_w_out, modD, 0, src0, dst0, "po0")

        hnA = nc.dram_tensor("hnAl", [S, D], F32, kind="Internal").ap()
        hnC = nc.dram_tensor("hnAc", [NCTX, D], F32, kind="Internal").ap()
        if not dbg.get("_skip_moe0"):
            phase_moe(nc, b, psum, 0, True, out, ctxB, hnA, hnC, router_w, w_gate, w_up, w_down, modD, ident, PV, dbg,
                      ada=None if skip_pre else (sT, ada_w, ada_b, modD))
            if not skip_pre:
                load_mod_vectors(nc, b, sb, modD, norm_mix_g, norm_ffn_g, PV, layers=(1,))
        elif not skip_pre:
            phase_adaln(nc, b, sb, stack, psum, cc, ada_w, ada_b, modD, ident, dbg, sT, layers=(1,))
            sch.barrier()
            load_mod_vectors(nc, b, sb, modD, norm_mix_g, norm_ffn_g, PV, layers=(1,))

        if dbg.get("_stop_l0"):
            pass
        else:
            oD = nc.dram_tensor("oD", [D, S], BF16, kind="Internal").ap()
            if not dbg.get("_skip_attn"):
                phase_attn(nc, b, psum, out, ctxB, w_qkv, q_gain, k_gain, sink, c_cos, c_sin, c_rot, c_mprev, c_mnext,
                           oD, ident, PV, dbg)

                def src1(j):
                    return (out[j * 128:(j + 1) * 128, :], "out%d" % j)

                phase_proj_out(nc, b, psum, oD, S, w_o, modD, 1, src1, src1, "po1")
            if not dbg.get("_skip_moe1"):
                phase_moe(nc, b, psum, 1, False, out, ctxB, hnA, hnC, router_w, w_gate, w_up, w_down, modD, ident, PV,
                          dbg)

        if dbg.get("vD") is not None:
            b.dma("sp", dbg["vD"], vD, ["vD"], ["dbgv"])
        if dbg.get("ctxB") is not None:
            b.dma("sp", dbg["ctxB"], ctxB, ["ctxB%d_%d" % (j, ds) for j in (16, 17) for ds in range(4)], ["dbgc"])
        if dbg.get("modD") is not None:
            sch.barrier()
            b.dma("sp", dbg["modD"], modD, ["modD"], ["dbg"])
        if dbg.get("pv") is not None:
            for i, n in enumerate(vec_names):
                b.dma("sp", dbg["pv"][i], PV[n][:], ["pv_" + n], ["dbg%d" % i])

        sch.barrier()
        sch.op("sp", lambda e: e.nop(), [], [])
        with nc.Block() as block:
            sch.emit(block)
    return nc


class NormT:
    def __init__(self, nc, b, st, ident, psum, pskeys, tag):
        self.nc, self.b, self.ident, self.psum, self.pskeys, self.tag = nc, b, ident, psum, pskeys, tag
        self.junk = st.enter_context(nc.sbuf_tensor(tag + "_junk", [128, D], BF16))
        self.sm = [st.enter_context(nc.sbuf_tensor(tag + "_sm%d" % i, [128, 4], F32)) for i in range(3)]
        self.n = 0
        self.n2 = 0

    def stage1(self, src, srckey, np_):
        b = self.b
        i = self.n % 3
        self.n += 1
        sm = self.sm[i]
        smk = "%s_sm%d" % (self.tag, i)
        jk = self.tag + "_junk"
        b.act(self.junk[0:np_, :], src, AF.Square, [srckey], [jk, smk + "a"], accum_out=sm[0:np_, 0:1])
        b.act(sm[0:np_, 1:2], sm[0:np_, 0:1], AF.Sqrt, [smk + "a"], [smk + "b"], scale=1.0 / D, bias=EPS)
        b.s.op("dve", lambda e: e.reciprocal(sm[0:np_, 2:3], sm[0:np_, 1:2]), [smk + "b"], [smk + "c"])
        b.ts("dve", src, src, sm[0:np_, 2:3], None, ALU.mult, None, [srckey, smk + "c"], [srckey])

    def stage2(self, src, srckey, np_, G, S, gkey, skey, out_fn, out_keys):
        b = self.b
        nb = len(self.psum) // 4
        base = (self.n2 % nb) * 4
        self.n2 += 1
        for k in range(16):
            pb = base + k // 4
            q = k % 4
            b.tr(self.psum[pb][:, q * 128:q * 128 + np_], src[:, k * 128:(k + 1) * 128],
                 self.ident[0:np_, 0:np_], [srckey, "ident"], [self.pskeys[pb]])
        for k in range(16):
            pb = base + k // 4
            q = k % 4
            pin = self.psum[pb][:, q * 128:q * 128 + np_]
            if (k // 4) % 2 == 1:
                b.act(out_fn(k), pin, AF.Identity, [self.pskeys[pb], gkey, skey], [out_keys(k)],
                      scale=G[:, k:k + 1], bias=S[:, k:k + 1])
            else:
                b.ts("dve", out_fn(k), pin, G[:, k:k + 1], S[:, k:k + 1], ALU.mult, ALU.add,
                     [self.pskeys[pb], gkey, skey], [out_keys(k)])

    def run(self, src, srckey, np_, G, S, gkey, skey, out_fn, out_keys):
        self.stage1(src, srckey, np_)
        self.stage2(src, srckey, np_, G, S, gkey, skey, out_fn, out_keys)


TOK_SLICES = [(0, 512), (512, 512), (1024, 512), (1536, 512), (2048, 256)]


def phase_conv_a(nc, b, psum, x, ctx, conv_w_in, conv_w, vD, ident, PV, dbg={}, ada=None):
    with contextlib.ExitStack() as st:
        def t(name, shape, dt):
            return st.enter_context(nc.sbuf_tensor(name, list(shape), dt))
        aT = t("ca_aT", [128, 16, NT], BF16)
        xt = [t("ca_xt%d" % i, [128, D], F32) for i in range(3)]
        NWS = 9
        wsm = [t("ca_w%d" % i, [128, 16, 128], BF16) for i in range(NWS)]
        u = [t("ca_u%d" % i, [128, NT], F32) for i in range(2)]
        y = [t("ca_y%d" % i, [128, NT], F32) for i in range(2)]
        csb = [t("ca_csb%d" % i, [128, 512], F32) for i in range(2)]
        vt = [t("ca_vt%d" % i, [128, NT], BF16) for i in range(2)]
        cw = t("ca_cw", [128, 3, 16], F32)
        for jj in range(3):
            b.dma("sp", cw[:, jj, :], conv_w[jj, :].rearrange("(k p) -> p k", p=128), [], ["ca_cw"],
                  allow_slow_non_contiguous=True)
        pskeys = ["ps%d" % i for i in range(8)]
        nt = NormT(nc, b, st, ident, psum, pskeys, "ca_nt")
        def s1(j):
            i = j % 3
            src = x[j * 128:(j + 1) * 128, :] if j < 16 else ctx[(j - 16) * 128:(j - 15) * 128, :]
            b.dma("sp", xt[i][:], src, [], ["ca_xt%d" % i])
            nt.stage1(xt[i][:], "ca_xt%d" % i, 128)

        def s2(j):
            i = j % 3
            r = "lat" if j < 16 else "ctx"
            nt.stage2(xt[i][:], "ca_xt%d" % i, 128, PV["G1_%s_0" % r], PV["S1_%s_0" % r],
                      "pv_G1_%s_0" % r, "pv_S1_%s_0" % r,
                      lambda k, j=j: aT[:, k, j * 128:(j + 1) * 128], lambda k, j=j: "ca_aT_%d_%d" % (j, k))

        s1(0)
        for j in range(18):
            if j + 1 < 18:
                s1(j + 1)
            s2(j)
        if dbg.get("aT") is not None:
            b.dma("sp", dbg["aT"], aT[:], ["ca_aT_%d_%d" % (j, k) for j in range(18) for k in range(16)], ["dbg_aT"])

        class _K:
            def __init__(self, pre):
                self.pre = pre

            def __getitem__(self, ok):
                o, k = ok
                n = dict(TOK_SLICES)[o]
                return [self.pre % (j, k) for j in range(o // 128, (o + n) // 128)]
        aT_keys = _K("ca_aT_%d_%d")
        cntc = {"npair": 0, "nb": 0}
        slots_of = {}

        def cx(ch):
            sl = {}
            for wi, (which, off) in enumerate((("c", D), ("x", 2 * D), ("b", 0))):
                si = (3 * ch + wi) % NWS
                b.dma("pool", wsm[si][:],
                      conv_w_in[:, off + ch * 128: off + (ch + 1) * 128].rearrange("(k p) f -> p k f", p=128),
                      [], ["ca_w%d" % si])
                sl[which] = si
            slots_of[ch] = sl
            ui = ch % 2
            for (o, n) in TOK_SLICES:
                pa = (cntc["npair"] % 2) * 2
                cntc["npair"] += 1
                for which, pb in (("c", pa), ("x", pa + 1)):
                    for k in range(16):
                        b.mm(psum[pb][:, 0:n], wsm[sl[which]][:, k, :], aT[:, k, o:o + n], k == 0, k == 15,
                             ["ca_w%d" % sl[which]] + aT_keys[o, k], ["ps%d" % pb])
                ci = cntc["npair"] % 2
                b.act(csb[ci][:, 0:n], psum[pa][:, 0:n], AF.Copy, ["ps%d" % pa], ["ca_csb%d" % ci])
                b.tt("dve", u[ui][:, o:o + n], csb[ci][:, 0:n], psum[pa + 1][:, 0:n], ALU.mult,
                     ["ca_csb%d" % ci, "ps%d" % (pa + 1)], ["ca_u%d_%d" % (ui, o)])
            ukeys = ["ca_u%d_%d" % (ui, o) for (o, n) in TOK_SLICES]
            yk = "ca_y%d" % ui
            b.ts("dve", y[ui][:], u[ui][:], cw[:, 1, ch:ch + 1], None, ALU.mult, None, ukeys + ["ca_cw"], [yk])
            for (lo, hi) in ((0, S), (S, NT)):
                b.stt("dve", y[ui][:, lo + 1:hi], u[ui][:, lo:hi - 1], cw[:, 0, ch:ch + 1], y[ui][:, lo + 1:hi],
                      ALU.mult, ALU.add, ukeys + ["ca_cw", yk], [yk])
                b.stt("dve", y[ui][:, lo:hi - 1], u[ui][:, lo + 1:hi], cw[:, 2, ch:ch + 1], y[ui][:, lo:hi - 1],
                      ALU.mult, ALU.add, ukeys + ["ca_cw", yk], [yk])
            if ch == 0 and dbg.get("u0") is not None:
                b.dma("sp", dbg["u0"], u[0][:], ukeys, ["dbg_u0"])
                b.dma("sp", dbg["y0"], y[0][:], [yk], ["dbg_y0"])

        def bpart(ch):
            sl = slots_of[ch]
            ui = ch % 2
            vi = ch % 2
            for (o, n) in TOK_SLICES:
                pb = 4 + cntc["nb"] % 2
                cntc["nb"] += 1
                for k in range(16):
                    b.mm(psum[pb][:, 0:n], wsm[sl["b"]][:, k, :], aT[:, k, o:o + n], k == 0, k == 15,
                         ["ca_w%d" % sl["b"]] + aT_keys[o, k], ["ps%d" % pb])
                b.tt("dve", vt[vi][:, o:o + n], psum[pb][:, 0:n], y[ui][:, o:o + n], ALU.mult,
                     ["ps%d" % pb, "ca_y%d" % ui], ["ca_vt%d_%d" % (vi, o)])
            b.dma("sp", vD[ch * 128:(ch + 1) * 128, :], vt[vi][:],
                  ["ca_vt%d_%d" % (vi, o) for (o, n) in TOK_SLICES], ["vD"])

        ada_n = [0]
        if ada is not None:
            sT, ada_w, ada_b, modD = ada
            aslot = [t("ca_adw%d" % i, [128, 16, 256], BF16) for i in range(2)]
            arow = [t("ca_adr%d" % i, [2, 256], F32) for i in range(2)]
            abias = [t("ca_adb%d" % i, [2, 256], F32) for i in range(2)]

        def ada_slots(k):
            if ada is None:
                return
            for _ in range(k):
                cs = ada_n[0]
                if cs >= 48:
                    return
                ada_n[0] += 1
                i = cs % 2
                c0 = cs * 256
                b.dma("pool", aslot[i][:], ada_w[1, :, c0:c0 + 256].rearrange("(k p) f -> p k f", p=128),
                      [], ["ca_adw%d" % i])
                b.dma("sp", abias[i][:], ada_b[1, c0:c0 + 256].partition_broadcast(2), [], ["ca_adb%d" % i])
                pb = 6 + i
                for k2 in range(16):
                    b.mm(psum[pb][0:2, 0:256], sT[:, k2, :], aslot[i][:, k2, :], k2 == 0, k2 == 15,
                         ["ad_sT", "ca_adw%d" % i], ["ps%d" % pb])
                b.tt("dve", arow[i][:, :], psum[pb][0:2, 0:256], abias[i][:, :], ALU.add,
                     ["ps%d" % pb, "ca_adb%d" % i], ["ca_adr%d" % i])
                b.dma("sp", modD[1, :, c0:c0 + 256], arow[i][:, :], ["ca_adr%d" % i], ["modD"])

        cx(0)
        ada_slots(3)
        for ch in range(1, 16):
            cx(ch)
            bpart(ch - 1)
            ada_slots(3)
        bpart(15)
        ada_slots(48)
    b.s.barrier()


def phase_proj_out(nc, b, psum, srcD, ntok, W, modD, l, res_src, res_dst, tag):
    with contextlib.ExitStack() as st:
        def t(name, shape, dt):
            return st.enter_context(nc.sbuf_tensor(name, list(shape), dt))
        vT = t(tag + "_vT", [128, 16, ntok], BF16)
        g1 = [t(tag + "_g1%d" % i, [128, D], F32) for i in range(2 if ntok > S else 1)]
        slots = [t(tag + "_w%d" % i, [128, 16, 512], BF16) for i in range(4)]
        for ds in range(4):
            b.dma("pool", slots[ds][:], W[:, ds * 512:(ds + 1) * 512].rearrange("(k p) f -> p k f", p=128),
                  [], [tag + "_w%d" % ds])
        xt = [t(tag + "_x%d" % i, [128, 512], F32) for i in range(3)]
        tm = [t(tag + "_t%d" % i, [128, 512], F32) for i in range(3)]
        for k in range(16):
            b.dma("sp", vT[:, k, :], srcD[k * 128:(k + 1) * 128, :], ["vD"], [tag + "_vT%d" % k])
        vkeys = [tag + "_vT%d" % k for k in range(16)]
        for i in range(len(g1)):
            b.dma("sp", g1[i][:], modD[l, i, 2 * D:3 * D].partition_broadcast(128), ["modD"], [tag + "_g1%d" % i])
        n = 0
        for ds in range(4):
            si = ds
            for j in range(ntok // 128):
                pb = n % 4
                i3 = n % 3
                n += 1
                gi = 0 if j < 16 else 1
                for k in range(16):
                    b.mm(psum[pb][:, :], vT[:, k, j * 128:(j + 1) * 128], slots[si][:, k, :], k == 0, k == 15,
                         vkeys + [tag + "_w%d" % si], ["ps%d" % pb])
                src, skey = res_src(j)
                dst, dkey = res_dst(j)
                b.dma("act", xt[i3][:], src[:, ds * 512:(ds + 1) * 512], [skey + "_%d" % ds], [tag + "_x%d" % i3])
                b.tt("dve", tm[i3][:], psum[pb][:, :], g1[gi][:, ds * 512:(ds + 1) * 512], ALU.mult,
                     ["ps%d" % pb, tag + "_g1%d" % gi], [tag + "_t%d" % i3])
                b.tt("pool", tm[i3][:], tm[i3][:], xt[i3][:], ALU.add,
                     [tag + "_t%d" % i3, tag + "_x%d" % i3], [tag + "_t%d" % i3])
                b.dma("sp", dst[:, ds * 512:(ds + 1) * 512], tm[i3][:], [tag + "_t%d" % i3], [dkey + "_%d" % ds])
    b.s.barrier()


class AdaStream:
    def __init__(self, nc, b, st, psum, banks, sT, ada_w, ada_b, modD, l, tag):
        self.b, self.psum, self.banks, self.sT, self.ada_w, self.ada_b, self.modD, self.l, self.tag = (
            b, psum, banks, sT, ada_w, ada_b, modD, l, tag)
        self.n = 0
        self.NS = 3
        self.slot = [st.enter_context(nc.sbuf_tensor(tag + "_w%d" % i, [128, 16, 256], BF16)) for i in range(self.NS)]
        self.row = [st.enter_context(nc.sbuf_tensor(tag + "_r%d" % i, [2, 256], F32)) for i in range(2)]
        self.bias = [st.enter_context(nc.sbuf_tensor(tag + "_b%d" % i, [2, 256], F32)) for i in range(2)]
        self.loaded = 0

    def _load(self):
        cs = self.loaded
        if cs >= 48:
            return
        self.loaded += 1
        i = cs % self.NS
        c0 = cs * 256
        self.b.dma("pool", self.slot[i][:],
                   self.ada_w[self.l, :, c0:c0 + 256].rearrange("(k p) f -> p k f", p=128), [],
                   [self.tag + "_w%d" % i])

    def slots(self, k):
        b, tag = self.b, self.tag
        for _ in range(k):
            cs = self.n
            if cs >= 48:
                return
            while self.loaded < min(48, cs + self.NS):
                self._load()
            self.n += 1
            i = cs % self.NS
            j = cs % 2
            c0 = cs * 256
            b.dma("sp", self.bias[j][:], self.ada_b[self.l, c0:c0 + 256].partition_broadcast(2), [],
                  [tag + "_b%d" % j])
            pb = self.banks[j]
            for k2 in range(16):
                b.mm(self.psum[pb][0:2, 0:256], self.sT[:, k2, :], self.slot[i][:, k2, :], k2 == 0, k2 == 15,
                     ["ad_sT", tag + "_w%d" % i], ["ps%d" % pb])
            b.tt("dve", self.row[j][:, :], self.psum[pb][0:2, 0:256], self.bias[j][:, :], ALU.add,
                 ["ps%d" % pb, tag + "_b%d" % j], [tag + "_r%d" % j])
            b.dma("sp", self.modD[self.l, :, c0:c0 + 256], self.row[j][:, :], [tag + "_r%d" % j], ["modD"])


def phase_moe(nc, b, psum, l, has_ctx, h_lat, h_ctx, hnA, hnC, router_w, w_gate, w_up, w_down, modD, ident, PV,
              dbg={}, ada=None):
    nch = 18 if has_ctx else 16
    ntok = CAP + (CAPC if has_ctx else 0)
    pskeys = ["ps%d" % i for i in range(8)]
    with contextlib.ExitStack() as st0:
        def t0(name, shape, dt):
            return st0.enter_context(nc.sbuf_tensor(name + "_L%d" % l, list(shape), dt))
        gateT = t0("mo_gateT", [128, 3, 16], F32)
        idxT = t0("mo_idxT", [128, 3, 16], I32)
        b.s.op("dve", lambda e: e.memset(gateT[:], 0.0), [], ["mo_gateT"])
        b.s.op("dve", lambda e: e.memset(idxT[:], 0), [], ["mo_idxT"])
        NW = 6
        wb = [t0("me_w%d" % i, [128, 8192], BF16) for i in range(NW)]

        def wview(si, kk, ff):
            return wb[si][:].rearrange("p (k f) -> p k f", k=kk)

        def slots_gu(e, half):
            if e >= NE:
                return
            for wi, W in enumerate((w_gate, w_up)):
                si = half * 2 + wi
                b.dma("pool", wview(si, 16, 512),
                      W[l, e, :, half * 512:(half + 1) * 512].rearrange("(k p) f -> p k f", p=128),
                      [], ["me_w%d" % si])

        def slots_d(e, dh):
            if e >= NE:
                return
            si = 4 + dh
            b.dma("pool", wview(si, 8, 1024),
                  w_down[l, e, :, dh * 1024:(dh + 1) * 1024].rearrange("(k p) f -> p k f", p=128),
                  [], ["me_w%d" % si])

        if not dbg.get("_stop_after_routing"):
            slots_gu(0, 0)
            slots_gu(0, 1)
            slots_d(0, 0)
            slots_d(0, 1)
        with contextlib.ExitStack() as st:
            def t(name, shape, dt):
                return st.enter_context(nc.sbuf_tensor(name + "_L%d" % l, list(shape), dt))
            ht = [t("mr_ht%d" % i, [128, D], F32) for i in range(3)]
            hmT = [t("mr_hmT%d" % i, [128, 16, 128], F32) for i in range(2)]
            wr = t("mr_wr", [128, 16, NE], F32)
            LTc = [t("mr_LTc%d" % i, [NE, 128], F32) for i in range(2)]
            L = t("mr_L", [128, 18, NE], F32)
            ex = t("mr_ex", [128, 18, NE], F32)
            aff = t("mr_aff", [128, 18, NE], F32)
            mx = t("mr_mx", [128, 18], F32)
            se = t("mr_se", [128, 18], F32)
            rs = t("mr_rs", [128, 18], F32)
            AffT = t("mr_AffT", [16, NT], F32)
            Wk = t("mr_Wk", [16, NT], F32)
            vals = t("mr_vals", [16, 288], F32)
            idxu = t("mr_idxu", [16, 288], U32)
            idxf = t("mr_idxf", [16, 288], F32)
            idxTf = t("mr_idxTf", [128, 3, 16], F32)
            b.dma("sp", wr[:], router_w[l].rearrange("(k p) e -> p k e", p=128), [], ["mr_wr"])
            nt = NormT(nc, b, st, ident, psum[0:4], pskeys[0:4], "mr_nt_L%d" % l)
            def rs1(j):
                i3 = j % 3
                lat = j < 16
                src = h_lat[j * 128:(j + 1) * 128, :] if lat else h_ctx[(j - 16) * 128:(j - 15) * 128, :]
                b.dma("sp", ht[i3][:], src, ["H_ALL"], ["mr_ht%d" % i3])
                nt.stage1(ht[i3][:], "mr_ht%d" % i3, 128)
                dst = hnA[j * 128:(j + 1) * 128, :] if lat else hnC[(j - 16) * 128:(j - 15) * 128, :]
                b.dma("sp", dst, ht[i3][:], ["mr_ht%d" % i3], ["hn_store%d" % j])

            rs1(0)
            for j in range(nch):
                if j + 1 < nch:
                    rs1(j + 1)
                i = j % 2
                i3 = j % 3
                r = "lat" if j < 16 else "ctx"
                nt.stage2(ht[i3][:], "mr_ht%d" % i3, 128, PV["G2_%s_%d" % (r, l)], PV["S2_%s_%d" % (r, l)],
                          "pv_G2_%s_%d" % (r, l), "pv_S2_%s_%d" % (r, l),
                          lambda k, i=i: hmT[i][:, k, :], lambda k, i=i: "mr_hmT%d_%d" % (i, k))
                pb = 4 + j % 2
                for k in range(16):
                    b.mm(psum[pb][0:NE, 0:128], wr[:, k, :], hmT[i][:, k, :], k == 0, k == 15,
                         ["mr_hmT%d_%d" % (i, k), "mr_wr"], [pskeys[pb]])
                b.cp("dve", LTc[i][:, :], psum[pb][0:NE, 0:128], [pskeys[pb]], ["mr_LTc%d" % i])
                b.tr(psum[pb][:, 256:256 + NE], LTc[i][:, :], ident[0:NE, 0:NE], ["mr_LTc%d" % i, "ident"], [pskeys[pb]])
                b.cp("dve", L[:, j, :], psum[pb][:, 256:256 + NE], [pskeys[pb]], ["mr_L%d" % j])
            Lk = ["mr_L%d" % j for j in range(nch)]
            if dbg.get("_rstage", 9) < 3:
                b.s.barrier()
                return
            b.s.op("dve", lambda e: e.tensor_reduce(mx[:, 0:nch], L[:, 0:nch, :], AX.X, ALU.max), Lk, ["mr_mx"])
            b.ts("dve", mx[:, 0:nch], mx[:, 0:nch], -1.0, None, ALU.mult, None, ["mr_mx"], ["mr_mx"])
            for j in range(nch):
                b.act(ex[:, j, :], L[:, j, :], AF.Exp, ["mr_L%d" % j, "mr_mx"], ["mr_ex%d" % j, "mr_se%d" % j],
                      bias=mx[:, j:j + 1], scale=1.0, accum_out=se[:, j:j + 1])
            b.s.op("dve", lambda e: e.reciprocal(rs[:, 0:nch], se[:, 0:nch]), ["mr_se%d" % j for j in range(nch)],
                   ["mr_rs"])
            for j in range(nch):
                b.ts("dve", aff[:, j, :], ex[:, j, :], rs[:, j:j + 1], None, ALU.mult, None,
                     ["mr_ex%d" % j, "mr_rs"], ["mr_aff%d" % j])
            if dbg.get("_rstage", 9) < 4:
                b.s.barrier()
                return
            for j in range(nch):
                pb = j // 4
                b.tr(psum[pb][0:16, (j % 4) * 128:(j % 4 + 1) * 128], aff[:, j, :], ident[:, :],
                     ["mr_aff%d" % j, "ident"], [pskeys[pb]])
            for pb in range((nch + 3) // 4):
                n = min(512, nch * 128 - pb * 512)
                b.cp("dve", AffT[:, pb * 512:pb * 512 + n], psum[pb][0:16, 0:n], [pskeys[pb]], ["mr_AffT%d" % pb])
                b.act(Wk[:, pb * 512:pb * 512 + n], AffT[:, pb * 512:pb * 512 + n], AF.Copy, ["mr_AffT%d" % pb], ["mr_Wk"])
            if dbg.get("aff") is not None and l == dbg.get("_l", 0):
                b.dma("sp", dbg["aff"][:, 0:nch * 128], AffT[:, 0:nch * 128],
                      ["mr_AffT%d" % pb for pb in range((nch + 3) // 4)], ["dbg_aff"])
            if dbg.get("_rstage", 9) < 5:
                b.s.barrier()
                return
            segs = [(0, S, 0, CAP // 8)] + ([(S, NT, CAP, CAPC // 8)] if has_ctx else [])
            adas = None
            if ada is not None:
                adas = AdaStream(nc, b, st, psum, (4, 5), ada[0], ada[1], ada[2], ada[3], 1, "mr_ada")
            for (lo, hi, vo, rounds) in segs:
                for r in range(rounds):
                    if adas is not None:
                        adas.slots(2 if r % 2 == 0 else 1)
                    vs = vals[:, vo + r * 8: vo + r * 8 + 8]
                    b.s.op("dve", lambda e, vs=vs, lo=lo, hi=hi: e.max(vs, Wk[:, lo:hi]), ["mr_Wk"], ["mr_vals"])
                    b.s.op("dve", lambda e, vs=vs, lo=lo, hi=hi, r=r, vo=vo: e.max_index(
                        idxu[:, vo + r * 8: vo + r * 8 + 8], vs, Wk[:, lo:hi]), ["mr_Wk", "mr_vals"], ["mr_idxu"])
                    if r < rounds - 1:
                        b.s.op("dve", lambda e, vs=vs, lo=lo, hi=hi: e.match_replace(Wk[:, lo:hi], vs, Wk[:, lo:hi], -1.0),
                               ["mr_Wk", "mr_vals", "mr_idxu"], ["mr_Wk"])
            if dbg.get("_rstage", 9) < 6:
                b.s.barrier()
                return
            if adas is not None:
                adas.slots(48)
            b.cp("dve", idxf[:, 0:ntok], idxu[:, 0:ntok], ["mr_idxu"], ["mr_idxf"])
            chunks = [(0, 128, 0), (128, 128, 1)] + ([(256, 32, 2)] if has_ctx else [])
            for (co, np_, cc) in chunks:
                b.tr(psum[6][0:np_, cc * 16:(cc + 1) * 16], vals[:, co:co + np_], ident[0:16, 0:16],
                     ["mr_vals", "ident"], [pskeys[6]])
                b.tr(psum[7][0:np_, cc * 16:(cc + 1) * 16], idxf[:, co:co + np_], ident[0:16, 0:16],
                     ["mr_idxf", "ident"], [pskeys[7]])
            for (co, np_, cc) in chunks:
                b.cp("dve", gateT[0:np_, cc, :], psum[6][0:np_, cc * 16:(cc + 1) * 16], [pskeys[6]], ["mo_gateT"])
                b.cp("dve", idxTf[0:np_, cc, :], psum[7][0:np_, cc * 16:(cc + 1) * 16], [pskeys[7]], ["mr_idxTf%d" % cc])
                b.cp("dve", idxT[0:np_, cc, :], idxTf[0:np_, cc, :], ["mr_idxTf%d" % cc], ["mo_idxT"])
            if dbg.get("idx") is not None and l == dbg.get("_l", 0):
                b.dma("sp", dbg["idx"][:, 0:ntok], idxu[:, 0:ntok], ["mr_idxu"], ["dbg_idx"])
                b.dma("sp", dbg["vals"][:, 0:ntok], vals[:, 0:ntok], ["mr_vals"], ["dbg_vals"])
            if dbg.get("idxT") is not None and l == dbg.get("_l", 0):
                b.dma("sp", dbg["idxT"], idxT[:], ["mo_idxT"], ["dbg_idxT"])
                b.dma("sp", dbg["gateT"], gateT[:], ["mo_gateT"], ["dbg_gateT"])
        b.s.barrier()
        if dbg.get("_stop_after_routing"):
            return
        with contextlib.ExitStack() as st:
            def t(name, shape, dt):
                return st.enter_context(nc.sbuf_tensor(name + "_L%d" % l, list(shape), dt))
            xs = [t("me_xs%d" % i, [128, D], F32) for i in range(3)]
            xsT = [t("me_xsT%d" % i, [128, 16, ntok], BF16) for i in range(2)]
            sa = [t("me_sa%d" % i, [128, ntok], F32) for i in range(2)]
            gT = [t("me_gT%d" % i, [128, 8, ntok], BF16) for i in range(2)]
            yo = [t("me_yo%d" % i, [128, D], F32) for i in range(3)]
            g2b = [t("me_g2b%d" % i, [128, D], F32) for i in range(2 if has_ctx else 1)]
            for i in range(len(g2b)):
                b.dma("sp", g2b[i][:], modD[l, i, 5 * D:6 * D].partition_broadcast(128), ["modD"], ["me_g2b%d" % i])
            chunks = [(0, 128, 0), (128, 128, 1)] + ([(256, 32, 2)] if has_ctx else [])
            cnt = {"xs": 0, "tp": 0, "pp": 0, "py": 0}

            gathered = {}

            def gathers(e):
                if e >= NE:
                    return
                for (co, np_, cc) in chunks:
                    i = cc
                    srcD = hnA if cc < 2 else hnC
                    ia = idxT[0:np_, cc, e:e + 1]
                    b.s.dma("pool", lambda eng, i=i, np_=np_, srcD=srcD, ia=ia: eng.indirect_dma_start(
                        out=xs[i][0:np_, :], out_offset=None, in_=srcD[:, :],
                        in_offset=bass.IndirectOffsetOnAxis(ap=ia, axis=0)),
                        ["mo_idxT", "HN"], ["me_xs%d" % i])
                    gathered[(e, cc)] = i

            def transposes(e):
                xi = e % 2
                for (co, np_, cc) in chunks:
                    i = gathered[(e, cc)]
                    r = "lat" if cc < 2 else "ctx"
                    G = PV["G2_%s_%d" % (r, l)]
                    Sv = PV["S2_%s_%d" % (r, l)]
                    gk, sk = "pv_G2_%s_%d" % (r, l), "pv_S2_%s_%d" % (r, l)
                    for kq in range(4):
                        pb = 6 + cnt["tp"] % 2
                        cnt["tp"] += 1
                        for q in range(4):
                            k = kq * 4 + q
                            b.tr(psum[pb][:, q * 128:q * 128 + np_], xs[i][0:np_, k * 128:(k + 1) * 128],
                                 ident[0:np_, 0:np_], ["me_xs%d" % i, "ident"], [pskeys[pb]])
                        for q in range(4):
                            k = kq * 4 + q
                            pin = psum[pb][:, q * 128:q * 128 + np_]
                            outp = xsT[xi][:, k, co:co + np_]
                            if kq % 2 == 1:
                                b.act(outp, pin, AF.Identity, [pskeys[pb], gk, sk], ["me_xsT%d_%d_%d" % (xi, cc, k)],
                                      scale=G[:, k:k + 1], bias=Sv[:, k:k + 1])
                            else:
                                b.ts("dve", outp, pin, G[:, k:k + 1], Sv[:, k:k + 1], ALU.mult, ALU.add,
                                     [pskeys[pb], gk, sk], ["me_xsT%d_%d_%d" % (xi, cc, k)])

            def xsT_keys(xi, k):
                return ["me_xsT%d_%d_%d" % (xi, cc, k) for (_, _, cc) in chunks]

            def gate_up(e):
                xi = e % 2
                gi = e % 2
                gathers(e + 1)
                for half in range(2):
                    sg, su = half * 2, half * 2 + 1
                    for fq in range(4):
                        fc = half * 4 + fq
                        pa = (cnt["pp"] % 2) * 2
                        cnt["pp"] += 1
                        for (si, pb) in ((sg, pa), (su, pa + 1)):
                            wv = wview(si, 16, 512)
                            for k in range(16):
                                b.mm(psum[pb][:, 0:ntok], wv[:, k, fq * 128:(fq + 1) * 128], xsT[xi][:, k, :],
                                     k == 0, k == 15, ["me_w%d" % si] + xsT_keys(xi, k), [pskeys[pb]])
                        s_i = cnt["pp"] % 2
                        b.act(sa[s_i][:, :], psum[pa][:, 0:ntok], AF.Silu, [pskeys[pa]], ["me_sa%d" % s_i])
                        b.tt("dve", gT[gi][:, fc, :], sa[s_i][:, :], psum[pa + 1][:, 0:ntok], ALU.mult,
                             ["me_sa%d" % s_i, pskeys[pa + 1]], ["me_gT%d_%d" % (gi, fc)])
                    slots_gu(e + 1, half)

            def down(e):
                gi = e % 2
                gk = ["me_gT%d_%d" % (gi, fc) for fc in range(8)]
                for dh in range(2):
                    si = 4 + dh
                    wv = wview(si, 8, 1024)
                    for (co, np_, cc) in chunks:
                        for dq in range(2):
                            ds = dh * 2 + dq
                            pb = 4 + cnt["py"] % 2
                            cnt["py"] += 1
                            for fk in range(8):
                                b.mm(psum[pb][0:np_, :], gT[gi][:, fk, co:co + np_], wv[:, fk, dq * 512:(dq + 1) * 512],
                                     fk == 0, fk == 7, gk + ["me_w%d" % si], [pskeys[pb]])
                            gb = g2b[0 if cc < 2 else 1]
                            b.stt("dve", yo[cc][0:np_, ds * 512:(ds + 1) * 512], psum[pb][0:np_, :],
                                  gateT[0:np_, cc, e:e + 1], gb[0:np_, ds * 512:(ds + 1) * 512], ALU.mult, ALU.mult,
                                  [pskeys[pb], "mo_gateT", "me_g2b%d" % (0 if cc < 2 else 1)],
                                  ["me_yo%d_%d" % (cc, ds)])
                    slots_d(e + 1, dh)

            def scatters(e):
                for (co, np_, cc) in chunks:
                    dstD = h_lat if cc < 2 else h_ctx
                    ia = idxT[0:np_, cc, e:e + 1]
                    b.s.dma("pool", lambda eng, np_=np_, cc=cc, dstD=dstD, ia=ia: eng.indirect_dma_start(
                        out=dstD[:, :], out_offset=bass.IndirectOffsetOnAxis(ap=ia, axis=0),
                        in_=yo[cc][0:np_, :], in_offset=None, compute_op=ALU.add),
                        ["mo_idxT"] + ["HS_%d_%d" % ((e + 1) % 2, c2) for c2 in range(3)]
                        + ["me_yo%d_%d" % (cc, ds) for ds in range(4)],
                        ["HS_%d_%d" % (e % 2, cc)])

            gathers(0)
            transposes(0)
            for e in range(NE):
                gate_up(e)
                down(e)
                if e + 1 < NE:
                    transposes(e + 1)
                scatters(e)
    b.s.barrier()


def phase_attn(nc, b, psum, h_lat, h_ctx, w_qkv, q_gain, k_gain, sink, c_cos, c_sin, c_rot, c_mprev, c_mnext,
               oD, ident, PV, dbg={}):
    pskeys = ["ps%d" % i for i in range(8)]
    with contextlib.ExitStack() as st:
        def t(name, shape, dt):
            return st.enter_context(nc.sbuf_tensor(name, list(shape), dt))
        aT = t("at_aT", [128, 16, NT], BF16)
        with contextlib.ExitStack() as st1:
            xt = [st1.enter_context(nc.sbuf_tensor("at_xt%d" % i, [128, D], F32)) for i in range(3)]
            nt = NormT(nc, b, st1, ident, psum, pskeys, "at_nt")

            def s1(j):
                i = j % 3
                src = h_lat[j * 128:(j + 1) * 128, :] if j < 16 else h_ctx[(j - 16) * 128:(j - 15) * 128, :]
                b.dma("sp", xt[i][:], src, ["H_ALL", "HC_ALL"], ["at_xt%d" % i])
                nt.stage1(xt[i][:], "at_xt%d" % i, 128)

            def s2(j):
                i = j % 3
                r = "lat" if j < 16 else "ctx"
                nt.stage2(xt[i][:], "at_xt%d" % i, 128, PV["G1_%s_1" % r], PV["S1_%s_1" % r],
                          "pv_G1_%s_1" % r, "pv_S1_%s_1" % r,
                          lambda k, j=j: aT[:, k, j * 128:(j + 1) * 128], lambda k, j=j: "at_aT_%d_%d" % (j, k))

            s1(0)
            for j in range(18):
                if j + 1 < 18:
                    s1(j + 1)
                s2(j)
        def aT_keys(o, k):
            n = dict(TOK_SLICES)[o]
            return ["at_aT_%d_%d" % (j, k) for j in range(o // 128, (o + n) // 128)]
        b.s.barrier()
        with contextlib.ExitStack() as st2:
            def t2(name, shape, dt):
                return st2.enter_context(nc.sbuf_tensor(name, list(shape), dt))
            qslot = t2("at_wq", [128, 16, 512], BF16)
            kslot = t2("at_wk", [128, 16, 128], BF16)
            vslot = t2("at_wv", [128, 16, 128], BF16)
            qT = t2("at_qT", [128, 4, S], BF16)
            kT = t2("at_kT", [128, NT], BF16)
            vv = t2("at_v", [128, 18, 128], BF16)
            cosT = t2("at_cos", [128, S], F32)
            sinT = t2("at_sin", [128, S], F32)
            oTg = t2("at_oTg", [128, 4, S], BF16)
            NB = 2
            sq = [t2("at_sq%d" % i, [128, 512], BF16) for i in range(3)]
            qf = [t2("at_qf%d" % i, [128, 512], F32) for i in range(3)]
            lnT = [t2("at_ln%d" % i, [128, 512], F32) for i in range(NB)]
            rstd = [t2("at_rstd%d" % i, [128, 512], F32) for i in range(NB)]
            qn = [t2("at_qn%d" % i, [128, 512], F32) for i in range(NB)]
            qnb = [t2("at_qnb%d" % i, [128, 512], BF16) for i in range(NB)]
            t1 = [t2("at_t1%d" % i, [128, 512], F32) for i in range(NB)]
            tt2 = [t2("at_t2%d" % i, [128, 512], F32) for i in range(NB)]
            pT = [t2("at_pT%d" % i, [128, 512], BF16) for i in range(5)]
            mprev = t2("at_mprev", [128, 512], BF16)
            mnext = t2("at_mnext", [128, 512], BF16)
            mtmp = t2("at_mtmp", [128, 512], F32)
            dsum = [t2("at_dsum%d" % i, [128, 512], F32) for i in range(2)]
            esink = t2("at_esink", [128, 16], F32)
            gq = t2("at_gq", [128, 1], F32)
            gk = t2("at_gk", [128, 1], F32)
            onesb = t2("at_ones", [128, 128], BF16)
            rotb = t2("at_rotb", [128, 128], BF16)
            rotf = t2("at_rotf", [128, 128], F32)
            b.dma("sp", cosT[:], c_cos, [], ["at_cos"])
            b.dma("sp", sinT[:], c_sin, [], ["at_sin"])
            b.dma("sp", rotf[:], c_rot, [], ["at_rotf"])
            b.cp("dve", rotb[:], rotf[:], ["at_rotf"], ["at_rotb"])
            b.s.op("dve", lambda e: e.memset(onesb[:], 1.0), [], ["at_ones"])
            b.dma("sp", mtmp[:], c_mprev, [], ["at_mtmp"])
            b.cp("dve", mprev[:], mtmp[:], ["at_mtmp"], ["at_mprev"])
            b.dma("sp", mtmp[:], c_mnext, ["at_mprev"], ["at_mtmp"])
            b.cp("dve", mnext[:], mtmp[:], ["at_mtmp"], ["at_mnext"])
            b.dma("sp", esink[:], sink.partition_broadcast(128), [], ["at_esink"])
            b.act(esink[:], esink[:], AF.Exp, ["at_esink"], ["at_esink"])
            b.dma("sp", gq[:], q_gain.rearrange("(p o) -> p o", o=1), [], ["at_gq"])
            b.dma("sp", gk[:], k_gain.rearrange("(p o) -> p o", o=1), [], ["at_gk"])
            b.ts("dve", gq[:], gq[:], float(128 ** -0.5), None, ALU.mult, None, ["at_gq"], ["at_gq"])
            cnt = {"pq": 0, "pv": 0, "ps": 0, "pt": 0, "nr": 0, "nr3": 0}
            epsT = t2("at_eps", [128, 1], F32)
            b.s.op("dve", lambda e: e.memset(epsT[:], EPS), [], ["at_eps"])

            def stage_a(tl):
                (wslot, wkey, c0, o, n, gain, gkey, rope, out_ap, out_key) = tl["p"]
                pb = cnt["pq"] % 2
                cnt["pq"] += 1
                w3 = cnt["nr3"] % 3
                cnt["nr3"] += 1
                tl["w3"] = w3
                for k in range(16):
                    b.mm(psum[pb][:, 0:n], wslot[:, k, c0:c0 + 128], aT[:, k, o:o + n], k == 0, k == 15,
                         [wkey] + aT_keys(o, k), [pskeys[pb]])
                b.act(sq[w3][:, 0:n], psum[pb][:, 0:n], AF.Square, [pskeys[pb]], ["at_sq%d" % w3])
                b.act(qf[w3][:, 0:n], psum[pb][:, 0:n], AF.Copy, [pskeys[pb]], ["at_qf%d" % w3])

            def stage_b(tl):
                (wslot, wkey, c0, o, n, gain, gkey, rope, out_ap, out_key) = tl["p"]
                w3 = tl["w3"]
                w = cnt["nr"] % NB
                cnt["nr"] += 1
                tl["w"] = w
                W = str(w)
                pss = 2 + w
                b.mm(psum[pss][:, 0:n], onesb[:, :], sq[w3][:, 0:n], True, True, ["at_ones", "at_sq%d" % w3], [pskeys[pss]])
                b.act(lnT[w][:, 0:n], psum[pss][:, 0:n], AF.Ln, [pskeys[pss], "at_eps"], ["at_ln" + W],
                      scale=1.0 / 128, bias=epsT[:, 0:1])
                b.act(rstd[w][:, 0:n], lnT[w][:, 0:n], AF.Exp, ["at_ln" + W], ["at_rstd" + W], scale=-0.5)
                if not rope:
                    b.stt("dve", out_ap, qf[w3][:, 0:n], gain[:, 0:1], rstd[w][:, 0:n], ALU.mult, ALU.mult,
                          ["at_qf%d" % w3, gkey, "at_rstd" + W], [out_key])
                    return
                b.stt("dve", qn[w][:, 0:n], qf[w3][:, 0:n], gain[:, 0:1], rstd[w][:, 0:n], ALU.mult, ALU.mult,
                      ["at_qf%d" % w3, gkey, "at_rstd" + W], ["at_qn" + W])
                b.act(qnb[w][:, 0:n], qn[w][:, 0:n], AF.Copy, ["at_qn" + W], ["at_qnb" + W])

            def stage_c(tl):
                (wslot, wkey, c0, o, n, gain, gkey, rope, out_ap, out_key) = tl["p"]
                if not rope:
                    return
                w = tl["w"]
                W = str(w)
                prot = 6 + w
                b.mm(psum[prot][:, 0:n], rotb[:, :], qnb[w][:, 0:n], True, True, ["at_rotb", "at_qnb" + W], [pskeys[prot]])
                b.tt("pool", t1[w][:, 0:n], qn[w][:, 0:n], cosT[:, o:o + n], ALU.mult,
                     ["at_qn" + W, "at_cos"], ["at_t1" + W])
                b.tt("dve", tt2[w][:, 0:n], psum[prot][:, 0:n], sinT[:, o:o + n], ALU.mult,
                     [pskeys[prot], "at_sin"], ["at_t2" + W])
                b.tt("dve", out_ap, t1[w][:, 0:n], tt2[w][:, 0:n], ALU.add, ["at_t1" + W, "at_t2" + W], [out_key])

            for g in range(4):
                b.dma("pool", qslot[:], w_qkv[:, g * 512:(g + 1) * 512].rearrange("(k p) f -> p k f", p=128),
                      [], ["at_wq"])
                b.dma("pool", kslot[:], w_qkv[:, 2048 + g * 128:2048 + (g + 1) * 128].rearrange("(k p) f -> p k f", p=128),
                      [], ["at_wk"])
                b.dma("pool", vslot[:], w_qkv[:, 2560 + g * 128:2560 + (g + 1) * 128].rearrange("(k p) f -> p k f", p=128),
                      [], ["at_wv"])
                tiles = []
                for (o, n) in TOK_SLICES:
                    tiles.append({"p": (kslot, "at_wk", 0, o, n, gk, "at_gk", o < S, kT[:, o:o + n], "at_kT_%d" % o)})
                for j in range(4):
                    for (o, n) in TOK_SLICES[0:4]:
                        tiles.append({"p": (qslot, "at_wq", j * 128, o, n, gq, "at_gq", True, qT[:, j, o:o + n],
                                            "at_qT_%d_%d" % (j, o))})
                nt_ = len(tiles)
                for sidx in range(nt_ + 2):
                    if sidx < nt_:
                        stage_a(tiles[sidx])
                    if 0 <= sidx - 1 < nt_:
                        stage_b(tiles[sidx - 1])
                    if 0 <= sidx - 2 < nt_:
                        stage_c(tiles[sidx - 2])
                for tcn in range(18):
                    pb = 4 + cnt["pv"] % 2
                    cnt["pv"] += 1
                    for k in range(16):
                        b.mm(psum[pb][:, 0:128], aT[:, k, tcn * 128:(tcn + 1) * 128], vslot[:, k, :], k == 0, k == 15,
                             ["at_wv", "at_aT_%d_%d" % (tcn, k)], [pskeys[pb]])
                    b.cp("dve", vv[:, tcn, :], psum[pb][:, 0:128], [pskeys[pb]], ["at_v_%d" % tcn])
                items = []
                for n in range(16):
                    chunks = []
                    if n > 0:
                        chunks.append(((n - 1) * 128, n - 1, "prev"))
                    chunks.append((n * 128, n, None))
                    if n < 15:
                        chunks.append(((n + 1) * 128, n + 1, "next"))
                    chunks.append((S, 16, None))
                    chunks.append((S + 128, 17, None))
                    for ci, (kc, vc, msk) in enumerate(chunks):
                        items.append((n, kc, vc, msk, ci == 0, ci == len(chunks) - 1))

                def score(it, idx):
                    n, kc, vc, msk, first, last = it
                    pb = (0, 1, 6)[idx % 3]
                    pi = idx % 5
                    qkeys = ["at_qT_%d_%d" % (j, (n // 4) * 512) for j in range(4)]
                    kkey = "at_kT_%d" % ((kc // 512) * 512 if kc < S else S)
                    b.mm(psum[pb][:, :], kT[:, kc:kc + 128], qT[:, :, n * 128:(n + 1) * 128], True, True,
                         [kkey] + qkeys, [pskeys[pb]])
                    b.act(pT[pi][:, :], psum[pb][:, :], AF.Exp, [pskeys[pb]], ["at_pT%d" % pi])
                    if msk is not None:
                        mt = mprev if msk == "prev" else mnext
                        b.tt("dve", pT[pi][:, :], pT[pi][:, :], mt[:, :], ALU.mult,
                             ["at_pT%d" % pi, "at_m" + msk], ["at_pT%d" % pi])

                def pv(it, idx):
                    n, kc, vc, msk, first, last = it
                    pi = idx % 5
                    po = 4 + n % 2
                    pd = 2 + n % 2
                    b.mm(psum[po][:, :], vv[:, vc, :], pT[pi][:, :], first, last,
                         ["at_v_%d" % vc, "at_pT%d" % pi], [pskeys[po]])
                    b.mm(psum[pd][:, :], onesb[:, :], pT[pi][:, :], first, last,
                         ["at_ones", "at_pT%d" % pi], [pskeys[pd]])
                    if last:
                        di = n % 2
                        for j in range(4):
                            h = 4 * g + j
                            b.ts("dve", dsum[di][:, j * 128:(j + 1) * 128], psum[pd][:, j * 128:(j + 1) * 128],
                                 esink[:, h:h + 1], None, ALU.add, None, [pskeys[pd], "at_esink"],
                                 ["at_dsum%d_%d" % (di, j), "at_dln%d" % di, "at_rden%d" % di])
                        b.act(dsum[di][:, :], dsum[di][:, :], AF.Ln, ["at_dsum%d_%d" % (di, j) for j in range(4)],
                              ["at_dln%d" % di])
                        b.act(dsum[di][:, :], dsum[di][:, :], AF.Exp, ["at_dln%d" % di], ["at_rden%d" % di], scale=-1.0)
                        b.tt("dve", oTg[:, :, n * 128:(n + 1) * 128], psum[po][:, :].rearrange("p (j q) -> p j q", j=4),
                             dsum[di][:, :].rearrange("p (j q) -> p j q", j=4), ALU.mult,
                             [pskeys[po], "at_rden%d" % di], ["at_oTg_%d" % n])

                LA = 3
                for idx, it in enumerate(items):
                    score(it, idx)
                    if idx >= LA:
                        pv(items[idx - LA], idx - LA)
                for idx in range(max(0, len(items) - LA), len(items)):
                    pv(items[idx], idx)
                for j in range(4):
                    h = 4 * g + j
                    b.dma("sp", oD[h * 128:(h + 1) * 128, :], oTg[:, j, :], ["at_oTg_%d" % n for n in range(16)],
                          ["oD%d" % h])
    b.s.barrier()


def phase_adaln(nc, b, sb, stack, psum, cc, ada_w, ada_b, modD, ident, dbg, sT, layers=(0, 1)):
    with contextlib.ExitStack() as st:
        def t(name, shape, dt):
            return st.enter_context(nc.sbuf_tensor(name + "_%d" % layers[0], list(shape), dt))
        cct = t("ad_cc", [2, D], F32)
        sil = t("ad_sil", [2, D], F32)
        slots = [t("ad_w%d" % i, [128, 16, 512], BF16) for i in range(3)]
        rows = [t("ad_row%d" % i, [2, 2048], F32) for i in range(2)]
        bias = [t("ad_b%d" % i, [2, 2048], F32) for i in range(2)]
        b.dma("sp", cct[:], cc, [], ["ad_cc"])
        b.act(sil[:], cct[:], AF.Silu, ["ad_cc"], ["ad_sil"])
        for k in range(16):
            b.tr(psum[0][:, 2 * k:2 * k + 2], sil[0:2, k * 128:(k + 1) * 128], ident[0:2, 0:2],
                 ["ad_sil", "ident"], ["ps0"])
        b.cp("dve", sT[:].rearrange("p k r -> p (k r)"), psum[0][:, 0:32], ["ps0"], ["ad_sT"])
        n = 0
        for l in layers:
            for g in range(6):
                rb = g % 2
                b.dma("sp", bias[rb][:], ada_b[l, g * 2048:(g + 1) * 2048].partition_broadcast(2),
                      [], ["ad_b%d" % rb])
                for q in range(4):
                    cs = g * 4 + q
                    si = n % 3
                    n += 1
                    src = ada_w[l, :, cs * 512:(cs + 1) * 512].rearrange("(k p) f -> p k f", p=128)
                    b.dma("pool", slots[si][:], src, [], ["ad_w%d" % si])
                    pb = 1 + (cs % 2)
                    for k in range(16):
                        b.mm(psum[pb][0:2, :], sT[:, k, :], slots[si][:, k, :], k == 0, k == 15,
                             ["ad_sT", "ad_w%d" % si], ["ps%d" % pb])
                    b.tt("dve", rows[rb][:, q * 512:(q + 1) * 512], psum[pb][0:2, :],
                         bias[rb][:, q * 512:(q + 1) * 512], ALU.add,
                         ["ps%d" % pb, "ad_b%d" % rb], ["ad_row%d_%d" % (rb, q)])
                b.dma("sp", modD[l, :, g * 2048:(g + 1) * 2048], rows[rb][:],
                      ["ad_row%d_%d" % (rb, q) for q in range(4)], ["modD"])


def load_mod_vectors(nc, b, sb, modD, norm_mix_g, norm_ffn_g, PV, layers=(0, 1)):
    with contextlib.ExitStack() as st:
        tmp_sc = st.enter_context(nc.sbuf_tensor("lm_sc%d" % layers[0], [128, 16], F32))
        tmp_g = st.enter_context(nc.sbuf_tensor("lm_g%d" % layers[0], [128, 16], F32))
        i = 0
        for l in layers:
            for gi, gsrc in ((1, norm_mix_g), (2, norm_ffn_g)):
                for ri, r in enumerate(("lat", "ctx")):
                    sh_off = 0 if gi == 1 else 3 * D
                    sc_off = D if gi == 1 else 4 * D
                    nS = "S%d_%s_%d" % (gi, r, l)
                    nG = "G%d_%s_%d" % (gi, r, l)
                    b.dma("sp", PV[nS][:], modD[l, ri, sh_off:sh_off + D].rearrange("(k p) -> p k", p=128),
                          ["modD"], ["pv_" + nS], allow_slow_non_contiguous=True)
                    b.dma("sp", tmp_sc[:], modD[l, ri, sc_off:sc_off + D].rearrange("(k p) -> p k", p=128),
                          ["modD"], ["lm_sc"], allow_slow_non_contiguous=True)
                    b.dma("sp", tmp_g[:], gsrc[l, :].rearrange("(k p) -> p k", p=128), [], ["lm_g"],
                          allow_slow_non_contiguous=True)
                    b.stt("dve", PV[nG][:], tmp_sc[:], 1.0, tmp_g[:], ALU.add, ALU.mult,
                          ["lm_sc", "lm_g"], ["pv_" + nG])
        b.s.barrier()


def _core_inputs(inputs, bi, consts):
    f = lambda a: np.ascontiguousarray(np.asarray(a), dtype=np.float32)
    m = {
        "x": f(inputs["x"][bi]),
        "ctx": f(inputs["ctx"][bi]),
        "cc": f(np.stack([np.asarray(inputs["c"][bi]), np.asarray(inputs["c_ctx"])])),
        "ada_w": f(inputs["ada_w"]),
        "ada_b": f(inputs["ada_b"]),
        "norm_mix_g": f(inputs["norm_mix_g"]),
        "norm_ffn_g": f(inputs["norm_ffn_g"]),
        "conv_w_in": f(inputs["conv_w_in"][0]),
        "conv_w": f(inputs["conv_w"][0]),
        "conv_w_out": f(inputs["conv_w_out"][0]),
        "attn_w_qkv": f(inputs["attn_w_qkv"][0]),
        "attn_q_gain": f(inputs["attn_q_gain"][0]),
        "attn_k_gain": f(inputs["attn_k_gain"][0]),
        "attn_sink": f(inputs["attn_sink"][0]),
        "attn_w_o": f(inputs["attn_w_o"][0]),
        "router_w": f(inputs["router_w"]),
        "expert_w_gate": f(inputs["expert_w_gate"]),
        "expert_w_up": f(inputs["expert_w_up"]),
        "expert_w_down": f(inputs["expert_w_down"]),
    }
    for k, v in consts.items():
        m["c_" + k] = v
    return m


def kernel(**inputs):
    nc = build_program()
    consts = _consts()
    shared = None
    in_maps = []
    for bi in range(NCORES):
        if shared is None:
            shared = _core_inputs(inputs, bi, consts)
            in_maps.append(shared)
        else:
            m = dict(shared)
            f = lambda a: np.ascontiguousarray(np.asarray(a), dtype=np.float32)
            m["x"] = f(inputs["x"][bi])
            m["ctx"] = f(inputs["ctx"][bi])
            m["cc"] = f(np.stack([np.asarray(inputs["c"][bi]), np.asarray(inputs["c_ctx"])]))
            in_maps.append(m)
    res = run_bass_kernel_spmd(nc, in_maps, core_ids=list(range(NCORES)))
    return np.stack([np.asarray(r["out"]) for r in res.results]).astype(np.float32)
```

```python
import contextlib
import numpy as np
import concourse.bass as bass
import concourse.mybir as mybir
from concourse.bass_utils import run_bass_kernel_spmd

F32 = mybir.dt.float32
BF16 = mybir.dt.bfloat16
I32 = mybir.dt.int32
U32 = mybir.dt.uint32
AF = mybir.ActivationFunctionType
ALU = mybir.AluOpType
AX = mybir.AxisListType

D = 2048
S = 2048
NCTX = 256
NT = S + NCTX
NE = 16
CAP = 256
CAPC = 32
FE = 1024
EPS = 1e-6
NCORES = 8


class Op:
    __slots__ = ("eng", "fn", "deps", "needs_inc", "dma", "sem", "val", "idx")

    def __init__(self, eng, fn, dma):
        self.eng = eng
        self.fn = fn
        self.dma = dma
        self.deps = set()
        self.needs_inc = dma
        self.sem = None
        self.val = 0


class Sched:
    ENGS = ("pe", "act", "dve", "pool", "sp")

    def __init__(self, nc, stack, n_dma_sems=(("sp", 8), ("pool", 12), ("act", 4))):
        self.nc = nc
        self.by_eng = {e: [] for e in self.ENGS}
        self.last_w = {}
        self.readers = {}
        self.prog_sem = {}
        for e in ("pe", "act", "dve", "pool"):
            self.prog_sem[e] = stack.enter_context(nc.semaphore("prog_" + e))
        self.dma_sems = {}
        self.dma_rr = {}
        self.dma_last = {}
        for q, n in n_dma_sems:
            self.dma_sems[q] = [stack.enter_context(nc.semaphore("dq_%s_%d" % (q, i))) for i in range(n)]
            self.dma_rr[q] = 0
        self.barrier_deps = {e: set() for e in self.ENGS}
        self.all_since_barrier = []
        self.nops = 0

    def _add(self, eng, fn, reads, writes, dma):
        o = Op(eng, fn, dma)
        self.nops += 1
        deps = o.deps
        for k in reads:
            w = self.last_w.get(k)
            if w is not None:
                deps.add(w)
        for k in writes:
            w = self.last_w.get(k)
            if w is not None:
                deps.add(w)
            rd = self.readers.get(k)
            if rd:
                for r in rd.values():
                    deps.add(r)
        if not dma:
            rset = None
            for d in list(deps):
                if (not d.dma) and d.eng == eng:
                    if eng == "pe":
                        deps.discard(d)
                    elif eng == "pool":
                        pass
                    else:
                        if rset is None:
                            rset = set()
                            for k in reads:
                                w = self.last_w.get(k)
                                if w is not None:
                                    rset.add(w)
                        if d not in rset:
                            deps.discard(d)
        bd = self.barrier_deps[eng]
        if bd:
            deps |= bd
            self.barrier_deps[eng] = set()
        if dma:
            sems = self.dma_sems[eng]
            i = self.dma_rr[eng]
            self.dma_rr[eng] = (i + 1) % len(sems)
            o.sem = sems[i]
            prev = self.dma_last.get((eng, i))
            if prev is not None:
                deps.add(prev)
                o.val = prev.val + 16
            else:
                o.val = 16
            self.dma_last[(eng, i)] = o
        for d in deps:
            d.needs_inc = True
        for k in writes:
            self.last_w[k] = o
            self.readers[k] = {}
        for k in reads:
            rd = self.readers.setdefault(k, {})
            rd[("dma", id(o)) if dma else eng] = o
        self.by_eng[eng].append(o)
        self.all_since_barrier.append(o)
        return o

    def op(self, eng, fn, reads=(), writes=()):
        return self._add(eng, fn, reads, writes, False)

    def dma(self, queue, fn, reads=(), writes=()):
        return self._add(queue, fn, reads, writes, True)

    def barrier(self):
        last = {}
        pend = set()
        for o in self.all_since_barrier:
            if o.dma:
                pend.add(o)
            else:
                last[o.eng] = o
        pend |= set(last.values())
        for e in self.ENGS:
            self.barrier_deps[e] |= pend
        for o in pend:
            o.needs_inc = True
        self.all_since_barrier = []

    def emit(self, block):
        nc = self.nc
        for e in ("pe", "act", "dve", "pool"):
            cnt = 0
            for o in self.by_eng[e]:
                if o.dma:
                    continue
                if o.needs_inc:
                    cnt += 1
                    o.sem = self.prog_sem[e]
                    o.val = cnt
        engmap = {"pe": "tensor", "act": "scalar", "dve": "vector", "pool": "gpsimd", "sp": "sync"}

        def run(e, eng):
            waited = {}
            for o in self.by_eng[e]:
                need = {}
                for d in o.deps:
                    key = id(d.sem)
                    if waited.get(key, 0) < d.val and need.get(key, (None, 0))[1] < d.val:
                        need[key] = (d.sem, d.val)
                for key, (sem, val) in need.items():
                    eng.wait_ge(sem, val)
                    waited[key] = val
                ins = o.fn(eng)
                if o.dma:
                    ins.then_inc(o.sem, 16)
                elif o.needs_inc:
                    ins.then_inc(o.sem, 1)

        for e in self.ENGS:
            if not self.by_eng[e]:
                continue
            deco = getattr(block, engmap[e])

            def body(eng, e=e):
                run(e, eng)

            deco(body)


def _consts():
    c = {}
    c["ident"] = np.eye(128, dtype=np.float32)
    t = np.arange(S)
    row = (t // 64).astype(np.float32)
    col = (t % 64).astype(np.float32)
    inv = (10000.0 ** (-np.arange(32, dtype=np.float32) * 2.0 / 64.0)).astype(np.float32)
    ang = np.zeros((128, S), np.float32)
    for i in range(128):
        pos = row if i < 64 else col
        ang[i] = pos * inv[i % 32]
    c["cosT"] = np.cos(ang).astype(np.float32)
    c["sinT"] = np.sin(ang).astype(np.float32)
    R = np.zeros((128, 128), np.float32)
    for m in range(128):
        if m % 64 < 32:
            R[m + 32, m] = -1.0
        else:
            R[m - 32, m] = 1.0
    c["rotm"] = R
    kk = np.arange(128)[:, None]
    qq = np.arange(128)[None, :]
    mprev = (kk >= qq).astype(np.float32)
    mnext = (kk <= qq).astype(np.float32)
    c["mprev"] = np.tile(mprev, (1, 4))
    c["mnext"] = np.tile(mnext, (1, 4))
    return c


class B:
    def __init__(self, nc, sch):
        self.nc = nc
        self.s = sch

    def mm(self, out, lhsT, rhs, start, stop, reads, writes):
        return self.s.op("pe", lambda e: e.matmul(out, lhsT, rhs, start=start, stop=stop), reads, writes)

    def tr(self, out, in_, ident, reads, writes):
        return self.s.op("pe", lambda e: e.transpose(out, in_, ident), reads, writes)

    def act(self, out, in_, func, reads, writes, **kw):
        return self.s.op("act", lambda e: e.activation(out, in_, func, **kw), reads, writes)

    def tt(self, eng, out, in0, in1, op, reads, writes):
        return self.s.op(eng, lambda e: e.tensor_tensor(out, in0, in1, op), reads, writes)

    def ts(self, eng, out, in0, s1, s2, op0, op1, reads, writes):
        if op1 is None:
            return self.s.op(eng, lambda e: e.tensor_scalar(out, in0, s1, None, op0), reads, writes)
        return self.s.op(eng, lambda e: e.tensor_scalar(out, in0, s1, s2, op0, op1), reads, writes)

    def stt(self, eng, out, in0, scalar, in1, op0, op1, reads, writes):
        return self.s.op(eng, lambda e: e.scalar_tensor_tensor(out, in0, scalar, in1, op0, op1), reads, writes)

    def cp(self, eng, out, in_, reads, writes):
        return self.s.op(eng, lambda e: e.tensor_copy(out, in_), reads, writes)

    def dma(self, q, out, in_, reads, writes, **kw):
        return self.s.dma(q, lambda e: e.dma_start(out=out, in_=in_, **kw), reads, writes)


def build_program(debug=None):
    nc = bass.Bass("TRN2", target_bir_lowering=False)
    dbg = {}

    tiny = ()
    for n_, v_, _ in (debug or []):
        if n_ == "_tiny":
            tiny = v_ if isinstance(v_, (list, tuple)) else (
                "ada_w", "conv_w_in", "conv_w_out", "attn_w_qkv", "attn_w_o", "expert_w_gate",
                "expert_w_up", "expert_w_down", "x", "ctx")

    def din(name, shape, dt=F32):
        if name in tiny:
            shape = [2] * len(shape)
        return nc.dram_tensor(name, list(shape), dt, kind="ExternalInput").ap()

    x = din("x", [S, D])
    ctx = din("ctx", [NCTX, D])
    cc = din("cc", [2, D])
    ada_w = din("ada_w", [2, D, 6 * D])
    ada_b = din("ada_b", [2, 6 * D])
    norm_mix_g = din("norm_mix_g", [2, D])
    norm_ffn_g = din("norm_ffn_g", [2, D])
    conv_w_in = din("conv_w_in", [D, 3 * D])
    conv_w = din("conv_w", [3, D])
    conv_w_out = din("conv_w_out", [D, D])
    w_qkv = din("attn_w_qkv", [D, 3072])
    q_gain = din("attn_q_gain", [128])
    k_gain = din("attn_k_gain", [128])
    sink = din("attn_sink", [16])
    w_o = din("attn_w_o", [D, D])
    router_w = din("router_w", [2, D, NE])
    w_gate = din("expert_w_gate", [2, NE, D, FE])
    w_up = din("expert_w_up", [2, NE, D, FE])
    w_down = din("expert_w_down", [2, NE, FE, D])
    c_ident = din("c_ident", [128, 128])
    c_cos = din("c_cosT", [128, S])
    c_sin = din("c_sinT", [128, S])
    c_rot = din("c_rotm", [128, 128])
    c_mprev = din("c_mprev", [128, 512])
    c_mnext = din("c_mnext", [128, 512])

    out = nc.dram_tensor("out", [S, D], F32, kind="ExternalOutput").ap()
    modD = nc.dram_tensor("modD", [2, 2, 6 * D], F32, kind="Internal").ap()
    hnA = nc.dram_tensor("hnA", [NT, D], F32, kind="Internal").ap()
    ctxB = nc.dram_tensor("ctxB", [NCTX, D], F32, kind="Internal").ap()
    vD = nc.dram_tensor("vD", [D, NT], BF16, kind="Internal").ap()

    if debug:
        for name, shape, dt in debug:
            if name.startswith("_"):
                dbg[name] = shape
                continue
            dbg[name] = nc.dram_tensor("dbg_" + name, list(shape), dt, kind="ExternalOutput").ap()

    with contextlib.ExitStack() as stack:
        sch = Sched(nc, stack)
        b = B(nc, sch)
        ent = stack.enter_context

        def sb(name, shape, dt):
            return ent(nc.sbuf_tensor(name, list(shape), dt))

        ident = sb("ident", [128, 128], F32)
        identb = sb("identb", [128, 128], BF16)
        b.dma("sp", ident[:], c_ident, [], ["ident"])
        b.cp("dve", identb[:], ident[:], ["ident"], ["identb"])
        vec_names = []
        for l in range(2):
            for r in ("lat", "ctx"):
                for nm in ("G1", "S1", "G2", "S2"):
                    vec_names.append("%s_%s_%d" % (nm, r, l))
        PV = {n: sb("pv_" + n, [128, 16], F32) for n in vec_names}
        psum = [ent(nc.psum_tensor("ps%d" % i, [128, 512], F32)) for i in range(8)]

        skip_pre = dbg.get("_skip_pre", False)
        sT = sb("ad_sT", [128, 16, 2], BF16)
        if not skip_pre:
            phase_adaln(nc, b, sb, stack, psum, cc, ada_w, ada_b, modD, ident, dbg, sT, layers=(0,))
        sch.barrier()
        load_mod_vectors(nc, b, sb, modD, norm_mix_g, norm_ffn_g, PV, layers=(0,) if not skip_pre else (0, 1))

        if not skip_pre:
            phase_conv_a(nc, b, psum, x, ctx, conv_w_in, conv_w, vD, ident, PV, dbg)

        def src0(j):
            return (x[j * 128:(j + 1) * 128, :], "x%d" % j) if j < 16 else (ctx[(j - 16) * 128:(j - 15) * 128, :], "cx%d" % j)

        def dst0(j):
            return (out[j * 128:(j + 1) * 128, :], "out%d" % j) if j < 16 else (ctxB[(j - 16) * 128:(j - 15) * 128, :], "ctxB%d" % j)

        if not skip_pre:
            phase_proj_out(nc, b, psum, vD, NT, conv_w_out, modD, 0, src0, dst0, "po0")

        hnA = nc.dram_tensor("hnAl", [S, D], F32, kind="Internal").ap()
        hnC = nc.dram_tensor("hnAc", [NCTX, D], F32, kind="Internal").ap()
        if not dbg.get("_skip_moe0"):
            phase_moe(nc, b, psum, 0, True, out, ctxB, hnA, hnC, router_w, w_gate, w_up, w_down, modD, ident, PV, dbg,
                      ada=None if skip_pre else (sT, ada_w, ada_b, modD))
            if not skip_pre:
                load_mod_vectors(nc, b, sb, modD, norm_mix_g, norm_ffn_g, PV, layers=(1,))
        elif not skip_pre:
            phase_adaln(nc, b, sb, stack, psum, cc, ada_w, ada_b, modD, ident, dbg, sT, layers=(1,))
            sch.barrier()
            load_mod_vectors(nc, b, sb, modD, norm_mix_g, norm_ffn_g, PV, layers=(1,))

        if dbg.get("_stop_l0"):
            pass
        else:
            oD = nc.dram_tensor("oD", [D, S], BF16, kind="Internal").ap()
            if not dbg.get("_skip_attn"):
                phase_attn(nc, b, psum, out, ctxB, w_qkv, q_gain, k_gain, sink, c_cos, c_sin, c_rot, c_mprev, c_mnext,
                           oD, ident, PV, dbg)

                def src1(j):
                    return (out[j * 128:(j + 1) * 128, :], "out%d" % j)

                phase_proj_out(nc, b, psum, oD, S, w_o, modD, 1, src1, src1, "po1")
            if not dbg.get("_skip_moe1"):
                phase_moe(nc, b, psum, 1, False, out, ctxB, hnA, hnC, router_w, w_gate, w_up, w_down, modD, ident, PV,
                          dbg)

        if dbg.get("vD") is not None:
            b.dma("sp", dbg["vD"], vD, ["vD"], ["dbgv"])
        if dbg.get("ctxB") is not None:
            b.dma("sp", dbg["ctxB"], ctxB, ["ctxB%d_%d" % (j, ds) for j in (16, 17) for ds in range(4)], ["dbgc"])
        if dbg.get("modD") is not None:
            sch.barrier()
            b.dma("sp", dbg["modD"], modD, ["modD"], ["dbg"])
        if dbg.get("pv") is not None:
            for i, n in enumerate(vec_names):
                b.dma("sp", dbg["pv"][i], PV[n][:], ["pv_" + n], ["dbg%d" % i])

        sch.barrier()
        sch.op("sp", lambda e: e.nop(), [], [])
        with nc.Block() as block:
            sch.emit(block)
    return nc


class NormT:
    def __init__(self, nc, b, st, ident, psum, pskeys, tag):
        self.nc, self.b, self.ident, self.psum, self.pskeys, self.tag = nc, b, ident, psum, pskeys, tag
        self.junk = st.enter_context(nc.sbuf_tensor(tag + "_junk", [128, D], BF16))
        self.sm = [st.enter_context(nc.sbuf_tensor(tag + "_sm%d" % i, [128, 4], F32)) for i in range(3)]
        self.n = 0
        self.n2 = 0

    def stage1(self, src, srckey, np_):
        b = self.b
        i = self.n % 3
        self.n += 1
        sm = self.sm[i]
        smk = "%s_sm%d" % (self.tag, i)
        jk = self.tag + "_junk"
        b.act(self.junk[0:np_, :], src, AF.Square, [srckey], [jk, smk + "a"], accum_out=sm[0:np_, 0:1])
        b.act(sm[0:np_, 1:2], sm[0:np_, 0:1], AF.Sqrt, [smk + "a"], [smk + "b"], scale=1.0 / D, bias=EPS)
        b.s.op("dve", lambda e: e.reciprocal(sm[0:np_, 2:3], sm[0:np_, 1:2]), [smk + "b"], [smk + "c"])
        b.ts("dve", src, src, sm[0:np_, 2:3], None, ALU.mult, None, [srckey, smk + "c"], [srckey])

    def stage2(self, src, srckey, np_, G, S, gkey, skey, out_fn, out_keys):
        b = self.b
        nb = len(self.psum) // 4
        base = (self.n2 % nb) * 4
        self.n2 += 1
        for k in range(16):
            pb = base + k // 4
            q = k % 4
            b.tr(self.psum[pb][:, q * 128:q * 128 + np_], src[:, k * 128:(k + 1) * 128],
                 self.ident[0:np_, 0:np_], [srckey, "ident"], [self.pskeys[pb]])
        for k in range(16):
            pb = base + k // 4
            q = k % 4
            pin = self.psum[pb][:, q * 128:q * 128 + np_]
            if (k // 4) % 2 == 1:
                b.act(out_fn(k), pin, AF.Identity, [self.pskeys[pb], gkey, skey], [out_keys(k)],
                      scale=G[:, k:k + 1], bias=S[:, k:k + 1])
            else:
                b.ts("dve", out_fn(k), pin, G[:, k:k + 1], S[:, k:k + 1], ALU.mult, ALU.add,
                     [self.pskeys[pb], gkey, skey], [out_keys(k)])

    def run(self, src, srckey, np_, G, S, gkey, skey, out_fn, out_keys):
        self.stage1(src, srckey, np_)
        self.stage2(src, srckey, np_, G, S, gkey, skey, out_fn, out_keys)


TOK_SLICES = [(0, 512), (512, 512), (1024, 512), (1536, 512), (2048, 256)]


def phase_conv_a(nc, b, psum, x, ctx, conv_w_in, conv_w, vD, ident, PV, dbg={}, ada=None):
    with contextlib.ExitStack() as st:
        def t(name, shape, dt):
            return st.enter_context(nc.sbuf_tensor(name, list(shape), dt))
        aT = t("ca_aT", [128, 16, NT], BF16)
        xt = [t("ca_xt%d" % i, [128, D], F32) for i in range(3)]
        NWS = 9
        wsm = [t("ca_w%d" % i, [128, 16, 128], BF16) for i in range(NWS)]
        u = [t("ca_u%d" % i, [128, NT], F32) for i in range(2)]
        y = [t("ca_y%d" % i, [128, NT], F32) for i in range(2)]
        csb = [t("ca_csb%d" % i, [128, 512], F32) for i in range(2)]
        vt = [t("ca_vt%d" % i, [128, NT], BF16) for i in range(2)]
        cw = t("ca_cw", [128, 3, 16], F32)
        for jj in range(3):
            b.dma("sp", cw[:, jj, :], conv_w[jj, :].rearrange("(k p) -> p k", p=128), [], ["ca_cw"],
                  allow_slow_non_contiguous=True)
        pskeys = ["ps%d" % i for i in range(8)]
        nt = NormT(nc, b, st, ident, psum, pskeys, "ca_nt")
        def s1(j):
            i = j % 3
            src = x[j * 128:(j + 1) * 128, :] if j < 16 else ctx[(j - 16) * 128:(j - 15) * 128, :]
            b.dma("sp", xt[i][:], src, [], ["ca_xt%d" % i])
            nt.stage1(xt[i][:], "ca_xt%d" % i, 128)

        def s2(j):
            i = j % 3
            r = "lat" if j < 16 else "ctx"
            nt.stage2(xt[i][:], "ca_xt%d" % i, 128, PV["G1_%s_0" % r], PV["S1_%s_0" % r],
                      "pv_G1_%s_0" % r, "pv_S1_%s_0" % r,
                      lambda k, j=j: aT[:, k, j * 128:(j + 1) * 128], lambda k, j=j: "ca_aT_%d_%d" % (j, k))

        s1(0)
        for j in range(18):
            if j + 1 < 18:
                s1(j + 1)
            s2(j)
        if dbg.get("aT") is not None:
            b.dma("sp", dbg["aT"], aT[:], ["ca_aT_%d_%d" % (j, k) for j in range(18) for k in range(16)], ["dbg_aT"])

        class _K:
            def __init__(self, pre):
                self.pre = pre

            def __getitem__(self, ok):
                o, k = ok
                n = dict(TOK_SLICES)[o]
                return [self.pre % (j, k) for j in range(o // 128, (o + n) // 128)]
        aT_keys = _K("ca_aT_%d_%d")
        cntc = {"npair": 0, "nb": 0}
        slots_of = {}

        def cx(ch):
            sl = {}
            for wi, (which, off) in enumerate((("c", D), ("x", 2 * D), ("b", 0))):
                si = (3 * ch + wi) % NWS
                b.dma("pool", wsm[si][:],
                      conv_w_in[:, off + ch * 128: off + (ch + 1) * 128].rearrange("(k p) f -> p k f", p=128),
                      [], ["ca_w%d" % si])
                sl[which] = si
            slots_of[ch] = sl
            ui = ch % 2
            for (o, n) in TOK_SLICES:
                pa = (cntc["npair"] % 2) * 2
                cntc["npair"] += 1
                for which, pb in (("c", pa), ("x", pa + 1)):
                    for k in range(16):
                        b.mm(psum[pb][:, 0:n], wsm[sl[which]][:, k, :], aT[:, k, o:o + n], k == 0, k == 15,
                             ["ca_w%d" % sl[which]] + aT_keys[o, k], ["ps%d" % pb])
                ci = cntc["npair"] % 2
                b.act(csb[ci][:, 0:n], psum[pa][:, 0:n], AF.Copy, ["ps%d" % pa], ["ca_csb%d" % ci])
                b.tt("dve", u[ui][:, o:o + n], csb[ci][:, 0:n], psum[pa + 1][:, 0:n], ALU.mult,
                     ["ca_csb%d" % ci, "ps%d" % (pa + 1)], ["ca_u%d_%d" % (ui, o)])
            ukeys = ["ca_u%d_%d" % (ui, o) for (o, n) in TOK_SLICES]
            yk = "ca_y%d" % ui
            b.ts("dve", y[ui][:], u[ui][:], cw[:, 1, ch:ch + 1], None, ALU.mult, None, ukeys + ["ca_cw"], [yk])
            for (lo, hi) in ((0, S), (S, NT)):
                b.stt("dve", y[ui][:, lo + 1:hi], u[ui][:, lo:hi - 1], cw[:, 0, ch:ch + 1], y[ui][:, lo + 1:hi],
                      ALU.mult, ALU.add, ukeys + ["ca_cw", yk], [yk])
                b.stt("dve", y[ui][:, lo:hi - 1], u[ui][:, lo + 1:hi], cw[:, 2, ch:ch + 1], y[ui][:, lo:hi - 1],
                      ALU.mult, ALU.add, ukeys + ["ca_cw", yk], [yk])
            if ch == 0 and dbg.get("u0") is not None:
                b.dma("sp", dbg["u0"], u[0][:], ukeys, ["dbg_u0"])
                b.dma("sp", dbg["y0"], y[0][:], [yk], ["dbg_y0"])

        def bpart(ch):
            sl = slots_of[ch]
            ui = ch % 2
            vi = ch % 2
            for (o, n) in TOK_SLICES:
                pb = 4 + cntc["nb"] % 2
                cntc["nb"] += 1
                for k in range(16):
                    b.mm(psum[pb][:, 0:n], wsm[sl["b"]][:, k, :], aT[:, k, o:o + n], k == 0, k == 15,
                         ["ca_w%d" % sl["b"]] + aT_keys[o, k], ["ps%d" % pb])
                b.tt("dve", vt[vi][:, o:o + n], psum[pb][:, 0:n], y[ui][:, o:o + n], ALU.mult,
                     ["ps%d" % pb, "ca_y%d" % ui], ["ca_vt%d_%d" % (vi, o)])
            b.dma("sp", vD[ch * 128:(ch + 1) * 128, :], vt[vi][:],
                  ["ca_vt%d_%d" % (vi, o) for (o, n) in TOK_SLICES], ["vD"])

        ada_n = [0]
        if ada is not None:
            sT, ada_w, ada_b, modD = ada
            aslot = [t("ca_adw%d" % i, [128, 16, 256], BF16) for i in range(2)]
            arow = [t("ca_adr%d" % i, [2, 256], F32) for i in range(2)]
            abias = [t("ca_adb%d" % i, [2, 256], F32) for i in range(2)]

        def ada_slots(k):
            if ada is None:
                return
            for _ in range(k):
                cs = ada_n[0]
                if cs >= 48:
                    return
                ada_n[0] += 1
                i = cs % 2
                c0 = cs * 256
                b.dma("pool", aslot[i][:], ada_w[1, :, c0:c0 + 256].rearrange("(k p) f -> p k f", p=128),
                      [], ["ca_adw%d" % i])
                b.dma("sp", abias[i][:], ada_b[1, c0:c0 + 256].partition_broadcast(2), [], ["ca_adb%d" % i])
                pb = 6 + i
                for k2 in range(16):
                    b.mm(psum[pb][0:2, 0:256], sT[:, k2, :], aslot[i][:, k2, :], k2 == 0, k2 == 15,
                         ["ad_sT", "ca_adw%d" % i], ["ps%d" % pb])
                b.tt("dve", arow[i][:, :], psum[pb][0:2, 0:256], abias[i][:, :], ALU.add,
                     ["ps%d" % pb, "ca_adb%d" % i], ["ca_adr%d" % i])
                b.dma("sp", modD[1, :, c0:c0 + 256], arow[i][:, :], ["ca_adr%d" % i], ["modD"])

        cx(0)
        ada_slots(3)
        for ch in range(1, 16):
            cx(ch)
            bpart(ch - 1)
            ada_slots(3)
        bpart(15)
        ada_slots(48)
    b.s.barrier()


def phase_proj_out(nc, b, psum, srcD, ntok, W, modD, l, res_src, res_dst, tag):
    with contextlib.ExitStack() as st:
        def t(name, shape, dt):
            return st.enter_context(nc.sbuf_tensor(name, list(shape), dt))
        vT = t(tag + "_vT", [128, 16, ntok], BF16)
        g1 = [t(tag + "_g1%d" % i, [128, D], F32) for i in range(2 if ntok > S else 1)]
        slots = [t(tag + "_w%d" % i, [128, 16, 512], BF16) for i in range(4)]
        xt = [t(tag + "_x%d" % i, [128, 512], F32) for i in range(3)]
        tm = [t(tag + "_t%d" % i, [128, 512], F32) for i in range(3)]
        for k in range(16):
            b.dma("sp", vT[:, k, :], srcD[k * 128:(k + 1) * 128, :], ["vD"], [tag + "_vT%d" % k])
        vkeys = [tag + "_vT%d" % k for k in range(16)]
        for ds in range(4):
            b.dma("pool", slots[ds][:], W[:, ds * 512:(ds + 1) * 512].rearrange("(k p) f -> p k f", p=128),
                  vkeys if ds > 0 else [], [tag + "_w%d" % ds])
        for i in range(len(g1)):
            b.dma("sp", g1[i][:], modD[l, i, 2 * D:3 * D].partition_broadcast(128), ["modD"], [tag + "_g1%d" % i])
        n = 0
        for ds in range(4):
            si = ds
            for j in range(ntok // 128):
                pb = n % 4
                i3 = n % 3
                n += 1
                gi = 0 if j < 16 else 1
                for k in range(16):
                    b.mm(psum[pb][:, :], vT[:, k, j * 128:(j + 1) * 128], slots[si][:, k, :], k == 0, k == 15,
                         vkeys + [tag + "_w%d" % si], ["ps%d" % pb])
                src, skey = res_src(j)
                dst, dkey = res_dst(j)
                b.dma("act", xt[i3][:], src[:, ds * 512:(ds + 1) * 512], [skey + "_%d" % ds], [tag + "_x%d" % i3])
                b.tt("dve", tm[i3][:], psum[pb][:, :], g1[gi][:, ds * 512:(ds + 1) * 512], ALU.mult,
                     ["ps%d" % pb, tag + "_g1%d" % gi], [tag + "_t%d" % i3])
                b.tt("pool", tm[i3][:], tm[i3][:], xt[i3][:], ALU.add,
                     [tag + "_t%d" % i3, tag + "_x%d" % i3], [tag + "_t%d" % i3])
                b.dma("sp", dst[:, ds * 512:(ds + 1) * 512], tm[i3][:], [tag + "_t%d" % i3], [dkey + "_%d" % ds])
    b.s.barrier()


class AdaStream:
    def __init__(self, nc, b, st, psum, banks, sT, ada_w, ada_b, modD, l, tag):
        self.b, self.psum, self.banks, self.sT, self.ada_w, self.ada_b, self.modD, self.l, self.tag = (
            b, psum, banks, sT, ada_w, ada_b, modD, l, tag)
        self.n = 0
        self.NS = 3
        self.slot = [st.enter_context(nc.sbuf_tensor(tag + "_w%d" % i, [128, 16, 256], BF16)) for i in range(self.NS)]
        self.row = [st.enter_context(nc.sbuf_tensor(tag + "_r%d" % i, [2, 256], F32)) for i in range(2)]
        self.bias = [st.enter_context(nc.sbuf_tensor(tag + "_b%d" % i, [2, 256], F32)) for i in range(2)]
        self.loaded = 0

    def _load(self):
        cs = self.loaded
        if cs >= 48:
            return
        self.loaded += 1
        i = cs % self.NS
        c0 = cs * 256
        self.b.dma("pool", self.slot[i][:],
                   self.ada_w[self.l, :, c0:c0 + 256].rearrange("(k p) f -> p k f", p=128), [],
                   [self.tag + "_w%d" % i])

    def slots(self, k):
        b, tag = self.b, self.tag
        for _ in range(k):
            cs = self.n
            if cs >= 48:
                return
            while self.loaded < min(48, cs + self.NS):
                self._load()
            self.n += 1
            i = cs % self.NS
            j = cs % 2
            c0 = cs * 256
            b.dma("act", self.bias[j][:], self.ada_b[self.l, c0:c0 + 256].partition_broadcast(2), [],
                  [tag + "_b%d" % j])
            pb = self.banks[j]
            for k2 in range(16):
                b.mm(self.psum[pb][0:2, 0:256], self.sT[:, k2, :], self.slot[i][:, k2, :], k2 == 0, k2 == 15,
                     ["ad_sT", tag + "_w%d" % i], ["ps%d" % pb])
            b.tt("dve", self.row[j][:, :], self.psum[pb][0:2, 0:256], self.bias[j][:, :], ALU.add,
                 ["ps%d" % pb, tag + "_b%d" % j], [tag + "_r%d" % j])
            b.dma("sp", self.modD[self.l, :, c0:c0 + 256], self.row[j][:, :], [tag + "_r%d" % j], ["modD"])


def phase_moe(nc, b, psum, l, has_ctx, h_lat, h_ctx, hnA, hnC, router_w, w_gate, w_up, w_down, modD, ident, PV,
              dbg={}, ada=None):
    nch = 18 if has_ctx else 16
    ntok = CAP + (CAPC if has_ctx else 0)
    pskeys = ["ps%d" % i for i in range(8)]
    with contextlib.ExitStack() as st0:
        def t0(name, shape, dt):
            return st0.enter_context(nc.sbuf_tensor(name + "_L%d" % l, list(shape), dt))
        gateT = t0("mo_gateT", [128, 3, 16], F32)
        idxT = t0("mo_idxT", [128, 3, 16], I32)
        b.s.op("dve", lambda e: e.memset(gateT[:], 0.0), [], ["mo_gateT"])
        b.s.op("dve", lambda e: e.memset(idxT[:], 0), [], ["mo_idxT"])
        NW = 6
        wb = [t0("me_w%d" % i, [128, 8192], BF16) for i in range(NW)]

        def wview(si, kk, ff):
            return wb[si][:].rearrange("p (k f) -> p k f", k=kk)

        def slots_gu(e, half):
            if e >= NE:
                return
            for wi, W in enumerate((w_gate, w_up)):
                si = half * 2 + wi
                b.dma("pool", wview(si, 16, 512),
                      W[l, e, :, half * 512:(half + 1) * 512].rearrange("(k p) f -> p k f", p=128),
                      [], ["me_w%d" % si])

        def slots_d(e, dh):
            if e >= NE:
                return
            si = 4 + dh
            b.dma("pool", wview(si, 8, 1024),
                  w_down[l, e, :, dh * 1024:(dh + 1) * 1024].rearrange("(k p) f -> p k f", p=128),
                  [], ["me_w%d" % si])

        if not dbg.get("_stop_after_routing"):
            slots_gu(0, 0)
            slots_gu(0, 1)
            slots_d(0, 0)
            slots_d(0, 1)
        with contextlib.ExitStack() as st:
            def t(name, shape, dt):
                return st.enter_context(nc.sbuf_tensor(name + "_L%d" % l, list(shape), dt))
            ht = [t("mr_ht%d" % i, [128, D], F32) for i in range(3)]
            hmT = [t("mr_hmT%d" % i, [128, 16, 128], F32) for i in range(2)]
            wr = t("mr_wr", [128, 16, NE], F32)
            LTc = [t("mr_LTc%d" % i, [NE, 128], F32) for i in range(2)]
            L = t("mr_L", [128, 18, NE], F32)
            ex = t("mr_ex", [128, 18, NE], F32)
            aff = t("mr_aff", [128, 18, NE], F32)
            mx = t("mr_mx", [128, 18], F32)
            se = t("mr_se", [128, 18], F32)
            rs = t("mr_rs", [128, 18], F32)
            AffT = t("mr_AffT", [16, NT], F32)
            Wk = t("mr_Wk", [16, NT], F32)
            vals = t("mr_vals", [16, 288], F32)
            idxu = t("mr_idxu", [16, 288], U32)
            idxf = t("mr_idxf", [16, 288], F32)
            idxTf = t("mr_idxTf", [128, 3, 16], F32)
            b.dma("sp", wr[:], router_w[l].rearrange("(k p) e -> p k e", p=128), [], ["mr_wr"])
            nt = NormT(nc, b, st, ident, psum[0:4], pskeys[0:4], "mr_nt_L%d" % l)
            def rs1(j):
                i3 = j % 3
                lat = j < 16
                src = h_lat[j * 128:(j + 1) * 128, :] if lat else h_ctx[(j - 16) * 128:(j - 15) * 128, :]
                b.dma("sp", ht[i3][:], src, ["H_ALL"], ["mr_ht%d" % i3])
                nt.stage1(ht[i3][:], "mr_ht%d" % i3, 128)
                dst = hnA[j * 128:(j + 1) * 128, :] if lat else hnC[(j - 16) * 128:(j - 15) * 128, :]
                b.dma("pool", dst, ht[i3][:], ["mr_ht%d" % i3], ["hn_store%d" % j])

            rs1(0)
            for j in range(nch):
                if j + 1 < nch:
                    rs1(j + 1)
                i = j % 2
                i3 = j % 3
                r = "lat" if j < 16 else "ctx"
                nt.stage2(ht[i3][:], "mr_ht%d" % i3, 128, PV["G2_%s_%d" % (r, l)], PV["S2_%s_%d" % (r, l)],
                          "pv_G2_%s_%d" % (r, l), "pv_S2_%s_%d" % (r, l),
                          lambda k, i=i: hmT[i][:, k, :], lambda k, i=i: "mr_hmT%d_%d" % (i, k))
                pb = 4 + j % 2
                for k in range(16):
                    b.mm(psum[pb][0:NE, 0:128], wr[:, k, :], hmT[i][:, k, :], k == 0, k == 15,
                         ["mr_hmT%d_%d" % (i, k), "mr_wr"], [pskeys[pb]])
                b.cp("dve", LTc[i][:, :], psum[pb][0:NE, 0:128], [pskeys[pb]], ["mr_LTc%d" % i])
                b.tr(psum[pb][:, 256:256 + NE], LTc[i][:, :], ident[0:NE, 0:NE], ["mr_LTc%d" % i, "ident"], [pskeys[pb]])
                b.cp("dve", L[:, j, :], psum[pb][:, 256:256 + NE], [pskeys[pb]], ["mr_L%d" % j])
            Lk = ["mr_L%d" % j for j in range(nch)]
            if dbg.get("_rstage", 9) < 3:
                b.s.barrier()
                return
            b.s.op("dve", lambda e: e.tensor_reduce(mx[:, 0:nch], L[:, 0:nch, :], AX.X, ALU.max), Lk, ["mr_mx"])
            b.ts("dve", mx[:, 0:nch], mx[:, 0:nch], -1.0, None, ALU.mult, None, ["mr_mx"], ["mr_mx"])
            for j in range(nch):
                b.act(ex[:, j, :], L[:, j, :], AF.Exp, ["mr_L%d" % j, "mr_mx"], ["mr_ex%d" % j, "mr_se%d" % j],
                      bias=mx[:, j:j + 1], scale=1.0, accum_out=se[:, j:j + 1])
            b.s.op("dve", lambda e: e.reciprocal(rs[:, 0:nch], se[:, 0:nch]), ["mr_se%d" % j for j in range(nch)],
                   ["mr_rs"])
            for j in range(nch):
                b.ts("dve", aff[:, j, :], ex[:, j, :], rs[:, j:j + 1], None, ALU.mult, None,
                     ["mr_ex%d" % j, "mr_rs"], ["mr_aff%d" % j])
            if dbg.get("_rstage", 9) < 4:
                b.s.barrier()
                return
            for j in range(nch):
                pb = j // 4
                b.tr(psum[pb][0:16, (j % 4) * 128:(j % 4 + 1) * 128], aff[:, j, :], ident[:, :],
                     ["mr_aff%d" % j, "ident"], [pskeys[pb]])
            for pb in range((nch + 3) // 4):
                n = min(512, nch * 128 - pb * 512)
                b.cp("dve", AffT[:, pb * 512:pb * 512 + n], psum[pb][0:16, 0:n], [pskeys[pb]], ["mr_AffT%d" % pb])
                b.act(Wk[:, pb * 512:pb * 512 + n], AffT[:, pb * 512:pb * 512 + n], AF.Copy, ["mr_AffT%d" % pb], ["mr_Wk"])
            if dbg.get("aff") is not None and l == dbg.get("_l", 0):
                b.dma("sp", dbg["aff"][:, 0:nch * 128], AffT[:, 0:nch * 128],
                      ["mr_AffT%d" % pb for pb in range((nch + 3) // 4)], ["dbg_aff"])
            if dbg.get("_rstage", 9) < 5:
                b.s.barrier()
                return
            segs = [(0, S, 0, CAP // 8)] + ([(S, NT, CAP, CAPC // 8)] if has_ctx else [])
            adas = None
            if ada is not None:
                adas = AdaStream(nc, b, st, psum, (4, 5), ada[0], ada[1], ada[2], ada[3], 1, "mr_ada")
            for (lo, hi, vo, rounds) in segs:
                for r in range(rounds):
                    if adas is not None:
                        adas.slots(2 if r % 2 == 0 else 1)
                    vs = vals[:, vo + r * 8: vo + r * 8 + 8]
                    b.s.op("dve", lambda e, vs=vs, lo=lo, hi=hi: e.max(vs, Wk[:, lo:hi]), ["mr_Wk"], ["mr_vals"])
                    b.s.op("dve", lambda e, vs=vs, lo=lo, hi=hi, r=r, vo=vo: e.max_index(
                        idxu[:, vo + r * 8: vo + r * 8 + 8], vs, Wk[:, lo:hi]), ["mr_Wk", "mr_vals"], ["mr_idxu"])
                    if r < rounds - 1:
                        b.s.op("dve", lambda e, vs=vs, lo=lo, hi=hi: e.match_replace(Wk[:, lo:hi], vs, Wk[:, lo:hi], -1.0),
                               ["mr_Wk", "mr_vals", "mr_idxu"], ["mr_Wk"])
            if dbg.get("_rstage", 9) < 6:
                b.s.barrier()
                return
            if adas is not None:
                adas.slots(48)
            b.cp("dve", idxf[:, 0:ntok], idxu[:, 0:ntok], ["mr_idxu"], ["mr_idxf"])
            chunks = [(0, 128, 0), (128, 128, 1)] + ([(256, 32, 2)] if has_ctx else [])
            for (co, np_, cc) in chunks:
                b.tr(psum[6][0:np_, cc * 16:(cc + 1) * 16], vals[:, co:co + np_], ident[0:16, 0:16],
                     ["mr_vals", "ident"], [pskeys[6]])
                b.tr(psum[7][0:np_, cc * 16:(cc + 1) * 16], idxf[:, co:co + np_], ident[0:16, 0:16],
                     ["mr_idxf", "ident"], [pskeys[7]])
            for (co, np_, cc) in chunks:
                b.cp("dve", gateT[0:np_, cc, :], psum[6][0:np_, cc * 16:(cc + 1) * 16], [pskeys[6]], ["mo_gateT"])
                b.cp("dve", idxTf[0:np_, cc, :], psum[7][0:np_, cc * 16:(cc + 1) * 16], [pskeys[7]], ["mr_idxTf%d" % cc])
                b.cp("dve", idxT[0:np_, cc, :], idxTf[0:np_, cc, :], ["mr_idxTf%d" % cc], ["mo_idxT"])
            if dbg.get("idx") is not None and l == dbg.get("_l", 0):
                b.dma("sp", dbg["idx"][:, 0:ntok], idxu[:, 0:ntok], ["mr_idxu"], ["dbg_idx"])
                b.dma("sp", dbg["vals"][:, 0:ntok], vals[:, 0:ntok], ["mr_vals"], ["dbg_vals"])
            if dbg.get("idxT") is not None and l == dbg.get("_l", 0):
                b.dma("sp", dbg["idxT"], idxT[:], ["mo_idxT"], ["dbg_idxT"])
                b.dma("sp", dbg["gateT"], gateT[:], ["mo_gateT"], ["dbg_gateT"])
        b.s.barrier()
        if dbg.get("_stop_after_routing"):
            return
        with contextlib.ExitStack() as st:
            def t(name, shape, dt):
                return st.enter_context(nc.sbuf_tensor(name + "_L%d" % l, list(shape), dt))
            xs = [t("me_xs%d" % i, [128, D], F32) for i in range(3)]
            xsT = [t("me_xsT%d" % i, [128, 16, ntok], BF16) for i in range(2)]
            sa = [t("me_sa%d" % i, [128, ntok], F32) for i in range(2)]
            gT = [t("me_gT%d" % i, [128, 8, ntok], BF16) for i in range(2)]
            yo = [t("me_yo%d" % i, [128, D], F32) for i in range(3)]
            g2b = [t("me_g2b%d" % i, [128, D], F32) for i in range(2 if has_ctx else 1)]
            for i in range(len(g2b)):
                b.dma("sp", g2b[i][:], modD[l, i, 5 * D:6 * D].partition_broadcast(128), ["modD"], ["me_g2b%d" % i])
            chunks = [(0, 128, 0), (128, 128, 1)] + ([(256, 32, 2)] if has_ctx else [])
            cnt = {"xs": 0, "tp": 0, "pp": 0, "py": 0}

            gathered = {}

            def gathers(e):
                if e >= NE:
                    return
                for (co, np_, cc) in chunks:
                    i = cc
                    srcD = hnA if cc < 2 else hnC
                    ia = idxT[0:np_, cc, e:e + 1]
                    b.s.dma("pool", lambda eng, i=i, np_=np_, srcD=srcD, ia=ia: eng.indirect_dma_start(
                        out=xs[i][0:np_, :], out_offset=None, in_=srcD[:, :],
                        in_offset=bass.IndirectOffsetOnAxis(ap=ia, axis=0)),
                        ["mo_idxT", "HN"], ["me_xs%d" % i])
                    gathered[(e, cc)] = i

            def transposes(e):
                xi = e % 2
                for (co, np_, cc) in chunks:
                    i = gathered[(e, cc)]
                    r = "lat" if cc < 2 else "ctx"
                    G = PV["G2_%s_%d" % (r, l)]
                    Sv = PV["S2_%s_%d" % (r, l)]
                    gk, sk = "pv_G2_%s_%d" % (r, l), "pv_S2_%s_%d" % (r, l)
                    for kq in range(4):
                        pb = 6 + cnt["tp"] % 2
                        cnt["tp"] += 1
                        for q in range(4):
                            k = kq * 4 + q
                            b.tr(psum[pb][:, q * 128:q * 128 + np_], xs[i][0:np_, k * 128:(k + 1) * 128],
                                 ident[0:np_, 0:np_], ["me_xs%d" % i, "ident"], [pskeys[pb]])
                        for q in range(4):
                            k = kq * 4 + q
                            pin = psum[pb][:, q * 128:q * 128 + np_]
                            outp = xsT[xi][:, k, co:co + np_]
                            if kq % 2 == 1:
                                b.act(outp, pin, AF.Identity, [pskeys[pb], gk, sk], ["me_xsT%d_%d_%d" % (xi, cc, k)],
                                      scale=G[:, k:k + 1], bias=Sv[:, k:k + 1])
                            else:
                                b.ts("dve", outp, pin, G[:, k:k + 1], Sv[:, k:k + 1], ALU.mult, ALU.add,
                                     [pskeys[pb], gk, sk], ["me_xsT%d_%d_%d" % (xi, cc, k)])

            def xsT_keys(xi, k):
                return ["me_xsT%d_%d_%d" % (xi, cc, k) for (_, _, cc) in chunks]

            def gate_up(e):
                xi = e % 2
                gi = e % 2
                gathers(e + 1)
                for half in range(2):
                    sg, su = half * 2, half * 2 + 1
                    for fq in range(4):
                        fc = half * 4 + fq
                        pa = (cnt["pp"] % 2) * 2
                        cnt["pp"] += 1
                        for (si, pb) in ((sg, pa), (su, pa + 1)):
                            wv = wview(si, 16, 512)
                            for k in range(16):
                                b.mm(psum[pb][:, 0:ntok], wv[:, k, fq * 128:(fq + 1) * 128], xsT[xi][:, k, :],
                                     k == 0, k == 15, ["me_w%d" % si] + xsT_keys(xi, k), [pskeys[pb]])
                        s_i = cnt["pp"] % 2
                        b.act(sa[s_i][:, :], psum[pa][:, 0:ntok], AF.Silu, [pskeys[pa]], ["me_sa%d" % s_i])
                        b.tt("dve", gT[gi][:, fc, :], sa[s_i][:, :], psum[pa + 1][:, 0:ntok], ALU.mult,
                             ["me_sa%d" % s_i, pskeys[pa + 1]], ["me_gT%d_%d" % (gi, fc)])
                    slots_gu(e + 1, half)

            def down(e):
                gi = e % 2
                gk = ["me_gT%d_%d" % (gi, fc) for fc in range(8)]
                for dh in range(2):
                    si = 4 + dh
                    wv = wview(si, 8, 1024)
                    for (co, np_, cc) in chunks:
                        for dq in range(2):
                            ds = dh * 2 + dq
                            pb = 4 + cnt["py"] % 2
                            cnt["py"] += 1
                            for fk in range(8):
                                b.mm(psum[pb][0:np_, :], gT[gi][:, fk, co:co + np_], wv[:, fk, dq * 512:(dq + 1) * 512],
                                     fk == 0, fk == 7, gk + ["me_w%d" % si], [pskeys[pb]])
                            gb = g2b[0 if cc < 2 else 1]
                            b.stt("dve", yo[cc][0:np_, ds * 512:(ds + 1) * 512], psum[pb][0:np_, :],
                                  gateT[0:np_, cc, e:e + 1], gb[0:np_, ds * 512:(ds + 1) * 512], ALU.mult, ALU.mult,
                                  [pskeys[pb], "mo_gateT", "me_g2b%d" % (0 if cc < 2 else 1)],
                                  ["me_yo%d_%d" % (cc, ds)])
                    slots_d(e + 1, dh)

            def scatters(e):
                for (co, np_, cc) in chunks:
                    dstD = h_lat if cc < 2 else h_ctx
                    ia = idxT[0:np_, cc, e:e + 1]
                    b.s.dma("pool", lambda eng, np_=np_, cc=cc, dstD=dstD, ia=ia: eng.indirect_dma_start(
                        out=dstD[:, :], out_offset=bass.IndirectOffsetOnAxis(ap=ia, axis=0),
                        in_=yo[cc][0:np_, :], in_offset=None, compute_op=ALU.add),
                        ["mo_idxT"] + ["HS_%d_%d" % ((e + 1) % 2, c2) for c2 in range(3)]
                        + ["me_yo%d_%d" % (cc, ds) for ds in range(4)],
                        ["HS_%d_%d" % (e % 2, cc)])

            gathers(0)
            transposes(0)
            for e in range(NE):
                gate_up(e)
                down(e)
                if e + 1 < NE:
                    transposes(e + 1)
                scatters(e)
    b.s.barrier()


def phase_attn(nc, b, psum, h_lat, h_ctx, w_qkv, q_gain, k_gain, sink, c_cos, c_sin, c_rot, c_mprev, c_mnext,
               oD, ident, PV, dbg={}):
    pskeys = ["ps%d" % i for i in range(8)]
    with contextlib.ExitStack() as st:
        def t(name, shape, dt):
            return st.enter_context(nc.sbuf_tensor(name, list(shape), dt))
        aT = t("at_aT", [128, 16, NT], BF16)
        with contextlib.ExitStack() as st1:
            xt = [st1.enter_context(nc.sbuf_tensor("at_xt%d" % i, [128, D], F32)) for i in range(3)]
            nt = NormT(nc, b, st1, ident, psum, pskeys, "at_nt")

            def s1(j):
                i = j % 3
                src = h_lat[j * 128:(j + 1) * 128, :] if j < 16 else h_ctx[(j - 16) * 128:(j - 15) * 128, :]
                b.dma("sp", xt[i][:], src, ["H_ALL", "HC_ALL"], ["at_xt%d" % i])
                nt.stage1(xt[i][:], "at_xt%d" % i, 128)

            def s2(j):
                i = j % 3
                r = "lat" if j < 16 else "ctx"
                nt.stage2(xt[i][:], "at_xt%d" % i, 128, PV["G1_%s_1" % r], PV["S1_%s_1" % r],
                          "pv_G1_%s_1" % r, "pv_S1_%s_1" % r,
                          lambda k, j=j: aT[:, k, j * 128:(j + 1) * 128], lambda k, j=j: "at_aT_%d_%d" % (j, k))

            s1(0)
            for j in range(18):
                if j + 1 < 18:
                    s1(j + 1)
                s2(j)
        def aT_keys(o, k):
            n = dict(TOK_SLICES)[o]
            return ["at_aT_%d_%d" % (j, k) for j in range(o // 128, (o + n) // 128)]
        b.s.barrier()
        with contextlib.ExitStack() as st2:
            def t2(name, shape, dt):
                return st2.enter_context(nc.sbuf_tensor(name, list(shape), dt))
            qslot = t2("at_wq", [128, 16, 512], BF16)
            kslot = t2("at_wk", [128, 16, 128], BF16)
            vslot = t2("at_wv", [128, 16, 128], BF16)
            qT = t2("at_qT", [128, 4, S], BF16)
            kT = t2("at_kT", [128, NT], BF16)
            vv = t2("at_v", [128, 18, 128], BF16)
            cosT = t2("at_cos", [128, S], F32)
            sinT = t2("at_sin", [128, S], F32)
            oTg = t2("at_oTg", [128, 4, S], BF16)
            NB = 2
            sq = [t2("at_sq%d" % i, [128, 512], BF16) for i in range(3)]
            qf = [t2("at_qf%d" % i, [128, 512], F32) for i in range(3)]
            lnT = [t2("at_ln%d" % i, [128, 512], F32) for i in range(NB)]
            rstd = [t2("at_rstd%d" % i, [128, 512], F32) for i in range(NB)]
            qn = [t2("at_qn%d" % i, [128, 512], F32) for i in range(NB)]
            qnb = [t2("at_qnb%d" % i, [128, 512], BF16) for i in range(NB)]
            t1 = [t2("at_t1%d" % i, [128, 512], F32) for i in range(NB)]
            tt2 = [t2("at_t2%d" % i, [128, 512], F32) for i in range(NB)]
            pT = [t2("at_pT%d" % i, [128, 512], BF16) for i in range(5)]
            mprev = t2("at_mprev", [128, 512], BF16)
            mnext = t2("at_mnext", [128, 512], BF16)
            mtmp = t2("at_mtmp", [128, 512], F32)
            dsum = [t2("at_dsum%d" % i, [128, 512], F32) for i in range(2)]
            esink = t2("at_esink", [128, 16], F32)
            gq = t2("at_gq", [128, 1], F32)
            gk = t2("at_gk", [128, 1], F32)
            onesb = t2("at_ones", [128, 128], BF16)
            rotb = t2("at_rotb", [128, 128], BF16)
            rotf = t2("at_rotf", [128, 128], F32)
            b.dma("sp", cosT[:], c_cos, [], ["at_cos"])
            b.dma("sp", sinT[:], c_sin, [], ["at_sin"])
            b.dma("sp", rotf[:], c_rot, [], ["at_rotf"])
            b.cp("dve", rotb[:], rotf[:], ["at_rotf"], ["at_rotb"])
            b.s.op("dve", lambda e: e.memset(onesb[:], 1.0), [], ["at_ones"])
            b.dma("sp", mtmp[:], c_mprev, [], ["at_mtmp"])
            b.cp("dve", mprev[:], mtmp[:], ["at_mtmp"], ["at_mprev"])
            b.dma("sp", mtmp[:], c_mnext, ["at_mprev"], ["at_mtmp"])
            b.cp("dve", mnext[:], mtmp[:], ["at_mtmp"], ["at_mnext"])
            b.dma("sp", esink[:], sink.partition_broadcast(128), [], ["at_esink"])
            b.act(esink[:], esink[:], AF.Exp, ["at_esink"], ["at_esink"])
            b.dma("sp", gq[:], q_gain.rearrange("(p o) -> p o", o=1), [], ["at_gq"])
            b.dma("sp", gk[:], k_gain.rearrange("(p o) -> p o", o=1), [], ["at_gk"])
            b.ts("dve", gq[:], gq[:], float(128 ** -0.5), None, ALU.mult, None, ["at_gq"], ["at_gq"])
            cnt = {"pq": 0, "pv": 0, "ps": 0, "pt": 0, "nr": 0, "nr3": 0}
            epsT = t2("at_eps", [128, 1], F32)
            b.s.op("dve", lambda e: e.memset(epsT[:], EPS), [], ["at_eps"])

            def stage_a(tl):
                (wslot, wkey, c0, o, n, gain, gkey, rope, out_ap, out_key) = tl["p"]
                pb = cnt["pq"] % 2
                cnt["pq"] += 1
                w3 = cnt["nr3"] % 3
                cnt["nr3"] += 1
                tl["w3"] = w3
                for k in range(16):
                    b.mm(psum[pb][:, 0:n], wslot[:, k, c0:c0 + 128], aT[:, k, o:o + n], k == 0, k == 15,
                         [wkey] + aT_keys(o, k), [pskeys[pb]])
                b.act(sq[w3][:, 0:n], psum[pb][:, 0:n], AF.Square, [pskeys[pb]], ["at_sq%d" % w3])
                b.act(qf[w3][:, 0:n], psum[pb][:, 0:n], AF.Copy, [pskeys[pb]], ["at_qf%d" % w3])

            def stage_b(tl):
                (wslot, wkey, c0, o, n, gain, gkey, rope, out_ap, out_key) = tl["p"]
                w3 = tl["w3"]
                w = cnt["nr"] % NB
                cnt["nr"] += 1
                tl["w"] = w
                W = str(w)
                pss = 2 + w
                b.mm(psum[pss][:, 0:n], onesb[:, :], sq[w3][:, 0:n], True, True, ["at_ones", "at_sq%d" % w3], [pskeys[pss]])
                b.act(lnT[w][:, 0:n], psum[pss][:, 0:n], AF.Ln, [pskeys[pss], "at_eps"], ["at_ln" + W],
                      scale=1.0 / 128, bias=epsT[:, 0:1])
                b.act(rstd[w][:, 0:n], lnT[w][:, 0:n], AF.Exp, ["at_ln" + W], ["at_rstd" + W], scale=-0.5)
                if not rope:
                    b.stt("dve", out_ap, qf[w3][:, 0:n], gain[:, 0:1], rstd[w][:, 0:n], ALU.mult, ALU.mult,
                          ["at_qf%d" % w3, gkey, "at_rstd" + W], [out_key])
                    return
                b.stt("dve", qn[w][:, 0:n], qf[w3][:, 0:n], gain[:, 0:1], rstd[w][:, 0:n], ALU.mult, ALU.mult,
                      ["at_qf%d" % w3, gkey, "at_rstd" + W], ["at_qn" + W])
                b.act(qnb[w][:, 0:n], qn[w][:, 0:n], AF.Copy, ["at_qn" + W], ["at_qnb" + W])

            def stage_c(tl):
                (wslot, wkey, c0, o, n, gain, gkey, rope, out_ap, out_key) = tl["p"]
                if not rope:
                    return
                w = tl["w"]
                W = str(w)
                prot = 6 + w
                b.mm(psum[prot][:, 0:n], rotb[:, :], qnb[w][:, 0:n], True, True, ["at_rotb", "at_qnb" + W], [pskeys[prot]])
                b.tt("pool", t1[w][:, 0:n], qn[w][:, 0:n], cosT[:, o:o + n], ALU.mult,
                     ["at_qn" + W, "at_cos"], ["at_t1" + W])
                b.tt("dve", tt2[w][:, 0:n], psum[prot][:, 0:n], sinT[:, o:o + n], ALU.mult,
                     [pskeys[prot], "at_sin"], ["at_t2" + W])
                b.tt("dve", out_ap, t1[w][:, 0:n], tt2[w][:, 0:n], ALU.add, ["at_t1" + W, "at_t2" + W], [out_key])

            for g in range(4):
                b.dma("pool", qslot[:], w_qkv[:, g * 512:(g + 1) * 512].rearrange("(k p) f -> p k f", p=128),
                      [], ["at_wq"])
                b.dma("pool", kslot[:], w_qkv[:, 2048 + g * 128:2048 + (g + 1) * 128].rearrange("(k p) f -> p k f", p=128),
                      [], ["at_wk"])
                b.dma("pool", vslot[:], w_qkv[:, 2560 + g * 128:2560 + (g + 1) * 128].rearrange("(k p) f -> p k f", p=128),
                      [], ["at_wv"])
                tiles = []
                for (o, n) in TOK_SLICES:
                    tiles.append({"p": (kslot, "at_wk", 0, o, n, gk, "at_gk", o < S, kT[:, o:o + n], "at_kT_%d" % o)})
                for j in range(4):
                    for (o, n) in TOK_SLICES[0:4]:
                        tiles.append({"p": (qslot, "at_wq", j * 128, o, n, gq, "at_gq", True, qT[:, j, o:o + n],
                                            "at_qT_%d_%d" % (j, o))})
                nt_ = len(tiles)
                for sidx in range(nt_ + 2):
                    if sidx < nt_:
                        stage_a(tiles[sidx])
                    if 0 <= sidx - 1 < nt_:
                        stage_b(tiles[sidx - 1])
                    if 0 <= sidx - 2 < nt_:
                        stage_c(tiles[sidx - 2])
                for tcn in range(18):
                    pb = 4 + cnt["pv"] % 2
                    cnt["pv"] += 1
                    for k in range(16):
                        b.mm(psum[pb][:, 0:128], aT[:, k, tcn * 128:(tcn + 1) * 128], vslot[:, k, :], k == 0, k == 15,
                             ["at_wv", "at_aT_%d_%d" % (tcn, k)], [pskeys[pb]])
                    b.cp("dve", vv[:, tcn, :], psum[pb][:, 0:128], [pskeys[pb]], ["at_v_%d" % tcn])
                items = []
                for n in range(16):
                    chunks = []
                    if n > 0:
                        chunks.append(((n - 1) * 128, n - 1, "prev"))
                    chunks.append((n * 128, n, None))
                    if n < 15:
                        chunks.append(((n + 1) * 128, n + 1, "next"))
                    chunks.append((S, 16, None))
                    chunks.append((S + 128, 17, None))
                    for ci, (kc, vc, msk) in enumerate(chunks):
                        items.append((n, kc, vc, msk, ci == 0, ci == len(chunks) - 1))

                def score(it, idx):
                    n, kc, vc, msk, first, last = it
                    pb = (0, 1, 6)[idx % 3]
                    pi = idx % 5
                    qkeys = ["at_qT_%d_%d" % (j, (n // 4) * 512) for j in range(4)]
                    kkey = "at_kT_%d" % ((kc // 512) * 512 if kc < S else S)
                    b.mm(psum[pb][:, :], kT[:, kc:kc + 128], qT[:, :, n * 128:(n + 1) * 128], True, True,
                         [kkey] + qkeys, [pskeys[pb]])
                    b.act(pT[pi][:, :], psum[pb][:, :], AF.Exp, [pskeys[pb]], ["at_pT%d" % pi])
                    if msk is not None:
                        mt = mprev if msk == "prev" else mnext
                        b.tt("dve", pT[pi][:, :], pT[pi][:, :], mt[:, :], ALU.mult,
                             ["at_pT%d" % pi, "at_m" + msk], ["at_pT%d" % pi])

                def pv(it, idx):
                    n, kc, vc, msk, first, last = it
                    pi = idx % 5
                    po = 4 + n % 2
                    pd = 2 + n % 2
                    b.mm(psum[po][:, :], vv[:, vc, :], pT[pi][:, :], first, last,
                         ["at_v_%d" % vc, "at_pT%d" % pi], [pskeys[po]])
                    b.mm(psum[pd][:, :], onesb[:, :], pT[pi][:, :], first, last,
                         ["at_ones", "at_pT%d" % pi], [pskeys[pd]])
                    if last:
                        di = n % 2
                        for j in range(4):
                            h = 4 * g + j
                            b.ts("dve", dsum[di][:, j * 128:(j + 1) * 128], psum[pd][:, j * 128:(j + 1) * 128],
                                 esink[:, h:h + 1], None, ALU.add, None, [pskeys[pd], "at_esink"],
                                 ["at_dsum%d_%d" % (di, j), "at_dln%d" % di, "at_rden%d" % di])
                        b.act(dsum[di][:, :], dsum[di][:, :], AF.Ln, ["at_dsum%d_%d" % (di, j) for j in range(4)],
                              ["at_dln%d" % di])
                        b.act(dsum[di][:, :], dsum[di][:, :], AF.Exp, ["at_dln%d" % di], ["at_rden%d" % di], scale=-1.0)
                        b.tt("dve", oTg[:, :, n * 128:(n + 1) * 128], psum[po][:, :].rearrange("p (j q) -> p j q", j=4),
                             dsum[di][:, :].rearrange("p (j q) -> p j q", j=4), ALU.mult,
                             [pskeys[po], "at_rden%d" % di], ["at_oTg_%d" % n])

                LA = 3
                for idx, it in enumerate(items):
                    score(it, idx)
                    if idx >= LA:
                        pv(items[idx - LA], idx - LA)
                for idx in range(max(0, len(items) - LA), len(items)):
                    pv(items[idx], idx)
                for j in range(4):
                    h = 4 * g + j
                    b.dma("sp", oD[h * 128:(h + 1) * 128, :], oTg[:, j, :], ["at_oTg_%d" % n for n in range(16)],
                          ["oD%d" % h])
    b.s.barrier()


def phase_adaln(nc, b, sb, stack, psum, cc, ada_w, ada_b, modD, ident, dbg, sT, layers=(0, 1)):
    with contextlib.ExitStack() as st:
        def t(name, shape, dt):
            return st.enter_context(nc.sbuf_tensor(name + "_%d" % layers[0], list(shape), dt))
        cct = t("ad_cc", [2, D], F32)
        sil = t("ad_sil", [2, D], F32)
        slots = [t("ad_w%d" % i, [128, 16, 512], BF16) for i in range(3)]
        rows = [t("ad_row%d" % i, [2, 2048], F32) for i in range(2)]
        bias = [t("ad_b%d" % i, [2, 2048], F32) for i in range(2)]
        b.dma("sp", cct[:], cc, [], ["ad_cc"])
        b.act(sil[:], cct[:], AF.Silu, ["ad_cc"], ["ad_sil"])
        for k in range(16):
            b.tr(psum[0][:, 2 * k:2 * k + 2], sil[0:2, k * 128:(k + 1) * 128], ident[0:2, 0:2],
                 ["ad_sil", "ident"], ["ps0"])
        b.cp("dve", sT[:].rearrange("p k r -> p (k r)"), psum[0][:, 0:32], ["ps0"], ["ad_sT"])
        n = 0
        for l in layers:
            for g in range(6):
                rb = g % 2
                b.dma("sp", bias[rb][:], ada_b[l, g * 2048:(g + 1) * 2048].partition_broadcast(2),
                      [], ["ad_b%d" % rb])
                for q in range(4):
                    cs = g * 4 + q
                    si = n % 3
                    n += 1
                    src = ada_w[l, :, cs * 512:(cs + 1) * 512].rearrange("(k p) f -> p k f", p=128)
                    b.dma("pool", slots[si][:], src, [], ["ad_w%d" % si])
                    pb = 1 + (cs % 2)
                    for k in range(16):
                        b.mm(psum[pb][0:2, :], sT[:, k, :], slots[si][:, k, :], k == 0, k == 15,
                             ["ad_sT", "ad_w%d" % si], ["ps%d" % pb])
                    b.tt("dve", rows[rb][:, q * 512:(q + 1) * 512], psum[pb][0:2, :],
                         bias[rb][:, q * 512:(q + 1) * 512], ALU.add,
                         ["ps%d" % pb, "ad_b%d" % rb], ["ad_row%d_%d" % (rb, q)])
                b.dma("sp", modD[l, :, g * 2048:(g + 1) * 2048], rows[rb][:],
                      ["ad_row%d_%d" % (rb, q) for q in range(4)], ["modD"])


def load_mod_vectors(nc, b, sb, modD, norm_mix_g, norm_ffn_g, PV, layers=(0, 1)):
    with contextlib.ExitStack() as st:
        tmp_sc = st.enter_context(nc.sbuf_tensor("lm_sc%d" % layers[0], [128, 16], F32))
        tmp_g = st.enter_context(nc.sbuf_tensor("lm_g%d" % layers[0], [128, 16], F32))
        i = 0
        for l in layers:
            for gi, gsrc in ((1, norm_mix_g), (2, norm_ffn_g)):
                for ri, r in enumerate(("lat", "ctx")):
                    sh_off = 0 if gi == 1 else 3 * D
                    sc_off = D if gi == 1 else 4 * D
                    nS = "S%d_%s_%d" % (gi, r, l)
                    nG = "G%d_%s_%d" % (gi, r, l)
                    b.dma("sp", PV[nS][:], modD[l, ri, sh_off:sh_off + D].rearrange("(k p) -> p k", p=128),
                          ["modD"], ["pv_" + nS], allow_slow_non_contiguous=True)
                    b.dma("sp", tmp_sc[:], modD[l, ri, sc_off:sc_off + D].rearrange("(k p) -> p k", p=128),
                          ["modD"], ["lm_sc"], allow_slow_non_contiguous=True)
                    b.dma("sp", tmp_g[:], gsrc[l, :].rearrange("(k p) -> p k", p=128), [], ["lm_g"],
                          allow_slow_non_contiguous=True)
                    b.stt("dve", PV[nG][:], tmp_sc[:], 1.0, tmp_g[:], ALU.add, ALU.mult,
                          ["lm_sc", "lm_g"], ["pv_" + nG])
        b.s.barrier()


def _core_inputs(inputs, bi, consts):
    f = lambda a: np.ascontiguousarray(np.asarray(a), dtype=np.float32)
    m = {
        "x": f(inputs["x"][bi]),
        "ctx": f(inputs["ctx"][bi]),
        "cc": f(np.stack([np.asarray(inputs["c"][bi]), np.asarray(inputs["c_ctx"])])),
        "ada_w": f(inputs["ada_w"]),
        "ada_b": f(inputs["ada_b"]),
        "norm_mix_g": f(inputs["norm_mix_g"]),
        "norm_ffn_g": f(inputs["norm_ffn_g"]),
        "conv_w_in": f(inputs["conv_w_in"][0]),
        "conv_w": f(inputs["conv_w"][0]),
        "conv_w_out": f(inputs["conv_w_out"][0]),
        "attn_w_qkv": f(inputs["attn_w_qkv"][0]),
        "attn_q_gain": f(inputs["attn_q_gain"][0]),
        "attn_k_gain": f(inputs["attn_k_gain"][0]),
        "attn_sink": f(inputs["attn_sink"][0]),
        "attn_w_o": f(inputs["attn_w_o"][0]),
        "router_w": f(inputs["router_w"]),
        "expert_w_gate": f(inputs["expert_w_gate"]),
        "expert_w_up": f(inputs["expert_w_up"]),
        "expert_w_down": f(inputs["expert_w_down"]),
    }
    for k, v in consts.items():
        m["c_" + k] = v
    return m


def kernel(**inputs):
    nc = build_program()
    consts = _consts()
    shared = None
    in_maps = []
    for bi in range(NCORES):
        if shared is None:
            shared = _core_inputs(inputs, bi, consts)
            in_maps.append(shared)
        else:
            m = dict(shared)
            f = lambda a: np.ascontiguousarray(np.asarray(a), dtype=np.float32)
            m["x"] = f(inputs["x"][bi])
            m["ctx"] = f(inputs["ctx"][bi])
            m["cc"] = f(np.stack([np.asarray(inputs["c"][bi]), np.asarray(inputs["c_ctx"])]))
            in_maps.append(m)
    res = run_bass_kernel_spmd(nc, in_maps, core_ids=list(range(NCORES)))
    return np.stack([np.asarray(r["out"]) for r in res.results]).astype(np.float32)
```
